# Optimizing a Trainium2 kernel written in Bass

```python
import jax, jax.numpy as jnp
from jax import lax
import numpy as np

D_MODEL = 4096
BATCH = 2
SEQ = 4096
DEPTH = 2

GRID_W = 64
CTX_LEN = 256
HEAD_DIM = 128
POOL_WINDOWS = (2, 4, 8, 16)
POOL_GROUPS = 4
POOL_W = D_MODEL // 4
POOL_GROUP_DIM = POOL_W // POOL_GROUPS
NA_W = 3 * D_MODEL // 8
NA_HEADS = NA_W // HEAD_DIM
NA_ROWS = 8
NA_COLS = 16
HG_W = D_MODEL - POOL_W - NA_W
HG_HEADS = HG_W // HEAD_DIM
HG_CHUNK = 64
FORGET_EPS = 1e-20
MIX_W = POOL_W + NA_W + HG_W
IN_W = POOL_W + 3 * NA_W + 5 * HG_W
N_EXPERTS = 32
TOP_K = 4
D_EXPERT = D_MODEL // 8
SWIGLU_LIMIT = 7.0
SWIGLU_ALPHA = 1.702
ROPE_BASE = 10000.0
LN_EPS = 1e-6
DEEPNORM_ALPHA = (2 * DEPTH) ** 0.25
DEEPNORM_BETA = (8 * DEPTH) ** -0.25

kernel_name = 'hybrid_pool_na_hgrn2_moe_dit'


def layer_norm(x, g=None, b=None):
    xf = x.astype(jnp.float32)
    mu = jnp.mean(xf, -1, keepdims=True)
    var = jnp.mean(jnp.square(xf - mu), -1, keepdims=True)
    y = (xf - mu) * lax.rsqrt(var + LN_EPS)
    if g is not None:
        y = y * g.astype(jnp.float32) + b.astype(jnp.float32)
    return y.astype(x.dtype)


def adaln(cond, w, b):
    return jnp.split(jax.nn.silu(cond) @ w + b, 6, axis=-1)


def modulate(y, shift, scale):
    return y * (1 + scale) + shift


def heads(t):
    return t.reshape(t.shape[:-1] + (-1, HEAD_DIM))


def flip(t):
    return jnp.flip(t, axis=1)


def split_proj(p):
    widths = (POOL_W, NA_W, NA_W, NA_W, HG_W, HG_W, HG_W, HG_W, HG_W)
    offs = np.cumsum(widths)[:-1].tolist()
    return jnp.split(p, offs, axis=-1)


def multiscale_pool(u, pool_w, pool_scale):
    B, L, _ = u.shape
    uf = u.astype(jnp.float32)
    cs = jnp.pad(jnp.cumsum(uf, axis=1), ((0, 0), (1, 0), (0, 0)))
    t = jnp.arange(L)
    diffs = []
    for gi, w in enumerate(POOL_WINDOWS):
        lo = jnp.clip(t - w // 2, 0, L)
        hi = jnp.clip(t + (w - w // 2), 0, L)
        sl = slice(gi * POOL_GROUP_DIM, (gi + 1) * POOL_GROUP_DIM)
        csg = cs[..., sl]
        mean = (csg[:, hi] - csg[:, lo]) / (hi - lo).astype(jnp.float32)[None, :, None]
        diffs.append(mean - uf[..., sl])
    d = jnp.stack(diffs, axis=2)
    y = jnp.einsum('blgi,gio->blgo', d, pool_w.astype(jnp.float32)).reshape(B, L, POOL_W)
    return (y * pool_scale.astype(jnp.float32)).astype(u.dtype)


def axial_rope(t):
    L = t.shape[1]
    pos = jnp.arange(L)
    half = HEAD_DIM // 2
    inv = ROPE_BASE ** (-jnp.arange(0, half, 2, dtype=jnp.float32) / half)

    def rot(u, p):
        ang = p.astype(jnp.float32)[:, None] * inv[None]
        cos = jnp.cos(ang)[None, :, None].astype(u.dtype)
        sin = jnp.sin(ang)[None, :, None].astype(u.dtype)
        u1, u2 = jnp.split(u, 2, axis=-1)
        return jnp.concatenate([u1 * cos - u2 * sin, u1 * sin + u2 * cos], -1)

    return jnp.concatenate([rot(t[..., :half], pos // GRID_W), rot(t[..., half:], pos % GRID_W)], -1)


def neighborhood_attention(q, k, v, kc, vc, rpb):
    B, L, H, d = q.shape
    rows = L // GRID_W
    kr = min(NA_ROWS, rows)
    scale = HEAD_DIM ** -0.5
    qg = q.reshape(B, rows, GRID_W, H, d)
    kg = k.reshape(B, rows, GRID_W, H, d)
    vg = v.reshape(B, rows, GRID_W, H, d)
    cols = jnp.arange(GRID_W)
    c_idx = jnp.clip(cols - NA_COLS // 2, 0, GRID_W - NA_COLS)[:, None] + jnp.arange(NA_COLS)[None]
    c_off = c_idx - cols[:, None] + (NA_COLS - 1)
    rpb_cols = rpb[:, :, c_off]

    def row_block(r):
        rs = jnp.clip(r - kr // 2, 0, rows - kr)
        kb = lax.dynamic_slice_in_dim(kg, rs, kr, axis=1)[:, :, c_idx]
        vb = lax.dynamic_slice_in_dim(vg, rs, kr, axis=1)[:, :, c_idx]
        qr = lax.dynamic_index_in_dim(qg, r, axis=1, keepdims=False)
        r_off = rs + jnp.arange(kr) - r + (NA_ROWS - 1)
        bias = jnp.transpose(rpb_cols[:, r_off], (0, 2, 1, 3)).astype(jnp.float32)
        s_loc = jnp.einsum('bqhd,bkqjhd->bhqkj', qr, kb).astype(jnp.float32) * scale + bias
        s_ctx = jnp.einsum('bqhd,bchd->bhqc', qr, kc).astype(jnp.float32) * scale
        s = jnp.concatenate([s_loc.reshape(B, H, GRID_W, kr * NA_COLS), s_ctx], -1)
        p = jax.nn.softmax(s, axis=-1).astype(v.dtype)
        p_loc = p[..., :kr * NA_COLS].reshape(B, H, GRID_W, kr, NA_COLS)
        p_ctx = p[..., kr * NA_COLS:]
        return (jnp.einsum('bhqkj,bkqjhd->bqhd', p_loc, vb)
                + jnp.einsum('bhqc,bchd->bqhd', p_ctx, vc))

    out = lax.map(row_block, jnp.arange(rows))
    return jnp.moveaxis(out, 0, 1).reshape(B, L, H * d)


def context_attention(q, k, v):
    B, Lc, H, d = q.shape
    s = jnp.einsum('bqhd,bkhd->bhqk', q, k).astype(jnp.float32) * (HEAD_DIM ** -0.5)
    p = jax.nn.softmax(s, axis=-1).astype(v.dtype)
    return jnp.einsum('bhqk,bkhd->bqhd', p, v).reshape(B, Lc, H * d)


def hgrn_keys(f_raw, lb):
    z = heads(f_raw.astype(jnp.float32))
    lb = lb.reshape(HG_HEADS, HEAD_DIM)
    sig = jax.nn.sigmoid(z)
    f = lb + (1.0 - lb) * sig
    log_f = jnp.log(jnp.maximum(f, FORGET_EPS))
    k = (1.0 - lb) * (1.0 - sig)
    return k, log_f


def hgrn_scan(q, k, v, log_f, s0):
    B, L, H, dk = q.shape
    C = min(HG_CHUNK, L)
    n = L // C

    def chunks(t):
        return jnp.moveaxis(t.reshape((B, n, C) + t.shape[2:]), 1, 0)

    causal = jnp.tril(jnp.ones((C, C), dtype=bool))[None, :, :, None, None]

    def step(s, inp):
        qc, kc, vc, gc = inp
        b = jnp.cumsum(gc, axis=1)
        o_inter = jnp.einsum('bthk,bhkv->bthv', qc * jnp.exp(b), s)
        diff = jnp.where(causal, b[:, :, None] - b[:, None, :], 0.0)
        decay = jnp.where(causal, jnp.exp(diff), 0.0)
        a = jnp.einsum('bthk,bshk,btshk->bhts', qc, kc, decay)
        o_intra = jnp.einsum('bhts,bshv->bthv', a, vc)
        b_end = b[:, -1]
        s_new = (jnp.exp(b_end)[..., None] * s
                 + jnp.einsum('bshk,bshv->bhkv', kc * jnp.exp(b_end[:, None] - b), vc))
        return s_new, o_inter + o_intra

    s_fin, o = lax.scan(step, s0, (chunks(q), chunks(k), chunks(v), chunks(log_f)))
    return jnp.moveaxis(o, 0, 1).reshape(B, L, H, -1), s_fin


def hgrn_final_state(k, v, log_f):
    b = jnp.cumsum(log_f, axis=1)
    return jnp.einsum('bshk,bshv->bhkv', k * jnp.exp(b[:, -1:] - b), v)


def hgrn_readout(o, g, norm_g):
    o = o * lax.rsqrt(jnp.mean(o * o, -1, keepdims=True) + LN_EPS)
    o = o.reshape(o.shape[:2] + (HG_W,)) * norm_g.astype(jnp.float32)
    return (o * jax.nn.silu(g.astype(jnp.float32))).astype(g.dtype)


def token_mixers(px, pc, pool_w, pool_scale, rpb, lb_f, lb_b, norm_g, ctx_out):
    ux, qx, kx, vx, hqx, hfx, hbx, hix, hgx = split_proj(px)
    uc, qc, kc, vc, hqc, hfc, hbc, hic, hgc = split_proj(pc)
    B = px.shape[0]
    a_x = multiscale_pool(ux, pool_w, pool_scale)
    kc_h, vc_h = heads(kc), heads(vc)
    b_x = neighborhood_attention(axial_rope(heads(qx)), axial_rope(heads(kx)), heads(vx), kc_h, vc_h, rpb)
    v_x, v_c = heads(hix.astype(jnp.float32)), heads(hic.astype(jnp.float32))
    q_x = heads(jax.nn.silu(hqx.astype(jnp.float32)))
    k_xf, g_xf = hgrn_keys(hfx, lb_f)
    k_xb, g_xb = hgrn_keys(hbx, lb_b)
    k_cf, g_cf = hgrn_keys(hfc, lb_f)
    k_cb, g_cb = hgrn_keys(hbc, lb_b)
    if ctx_out:
        q_c = heads(jax.nn.silu(hqc.astype(jnp.float32)))
        s0 = jnp.zeros((B, HG_HEADS, HEAD_DIM, HEAD_DIM), jnp.float32)
        o_cf, s_cf = hgrn_scan(q_c, k_cf, v_c, g_cf, s0)
        o_cb, s_cb = hgrn_scan(flip(q_c), flip(k_cb), flip(v_c), flip(g_cb), s0)
        c_c = hgrn_readout(o_cf + flip(o_cb), hgc, norm_g)
    else:
        s_cf = hgrn_final_state(k_cf, v_c, g_cf)
        s_cb = hgrn_final_state(flip(k_cb), flip(v_c), flip(g_cb))
    o_xf, _ = hgrn_scan(q_x, k_xf, v_x, g_xf, s_cf)
    o_xb, _ = hgrn_scan(flip(q_x), flip(k_xb), flip(v_x), flip(g_xb), s_cb)
    c_x = hgrn_readout(o_xf + flip(o_xb), hgx, norm_g)
    mix_x = jnp.concatenate([a_x, b_x, c_x], -1)
    if not ctx_out:
        return mix_x, None
    a_c = multiscale_pool(uc, pool_w, pool_scale)
    b_c = context_attention(heads(qc), kc_h, vc_h)
    mix_c = jnp.concatenate([a_c, b_c, c_c], -1)
    return mix_x, mix_c


def moe_ffn(t, rw, rb, w1, b1, w2, b2):
    logits = (t @ rw + rb).astype(jnp.float32)
    top_v, top_i = lax.top_k(logits, TOP_K)
    wts = jax.nn.softmax(top_v, axis=-1)
    gates = jnp.einsum('tk,tke->te', wts, jax.nn.one_hot(top_i, N_EXPERTS, dtype=jnp.float32)).astype(t.dtype)
    out = jnp.zeros_like(t)
    for e in range(N_EXPERTS):
        gate, up = jnp.split(t @ w1[e] + b1[e], 2, axis=-1)
        gate = jnp.minimum(gate, SWIGLU_LIMIT)
        up = jnp.clip(up, -SWIGLU_LIMIT, SWIGLU_LIMIT)
        hdn = (up + 1) * (gate * jax.nn.sigmoid(SWIGLU_ALPHA * gate))
        out = out + gates[:, e:e + 1] * (hdn @ w2[e] + b2[e])
    return out


def setup_inputs(seed: int = 0) -> dict:
    key = jax.random.key(seed)
    ks = jax.random.split(key, 24)
    f32 = jnp.float32

    def nrm(k, shape, scale):
        return jax.random.normal(k, shape, f32) * scale

    return {
        'x': nrm(ks[0], (BATCH, SEQ, D_MODEL), 1.0),
        'c': nrm(ks[1], (BATCH, D_MODEL), 1.0),
        'ctx': nrm(ks[2], (BATCH, CTX_LEN, D_MODEL), 1.0),
        'c_ctx': nrm(ks[3], (D_MODEL,), 1.0),
        'w_mod': nrm(ks[4], (DEPTH, D_MODEL, 6 * D_MODEL), 0.5 * D_MODEL ** -0.5),
        'b_mod': nrm(ks[5], (DEPTH, 6 * D_MODEL), 0.02),
        'w_in': nrm(ks[6], (DEPTH, D_MODEL, IN_W), D_MODEL ** -0.5),
        'pool_w': nrm(ks[7], (DEPTH, POOL_GROUPS, POOL_GROUP_DIM, POOL_GROUP_DIM), POOL_GROUP_DIM ** -0.5),
        'pool_scale': 1.0 + nrm(ks[8], (DEPTH, POOL_W), 0.1),
        'na_rpb': nrm(ks[9], (DEPTH, NA_HEADS, 2 * NA_ROWS - 1, 2 * NA_COLS - 1), 0.1),
        'hg_lb': nrm(ks[10], (2, DEPTH, HG_W), 1.0),
        'hg_norm_g': 1.0 + nrm(ks[11], (DEPTH, HG_W), 0.1),
        'w_out': nrm(ks[12], (DEPTH, MIX_W, D_MODEL), MIX_W ** -0.5 * DEEPNORM_BETA),
        'ln1_g': 1.0 + nrm(ks[13], (DEPTH, D_MODEL), 0.1),
        'ln1_b': nrm(ks[14], (DEPTH, D_MODEL), 0.02),
        'ln2_g': 1.0 + nrm(ks[15], (DEPTH, D_MODEL), 0.1),
        'ln2_b': nrm(ks[16], (DEPTH, D_MODEL), 0.02),
        'router_w': nrm(ks[17], (DEPTH, D_MODEL, N_EXPERTS), D_MODEL ** -0.5),
        'router_b': nrm(ks[18], (DEPTH, N_EXPERTS), 0.01),
        'exp_w1': nrm(ks[19], (DEPTH, N_EXPERTS, D_MODEL, 2 * D_EXPERT), D_MODEL ** -0.5),
        'exp_b1': nrm(ks[20], (DEPTH, N_EXPERTS, 2 * D_EXPERT), 0.01),
        'exp_w2': nrm(ks[21], (DEPTH, N_EXPERTS, D_EXPERT, D_MODEL), D_EXPERT ** -0.5 * DEEPNORM_BETA),
        'exp_b2': nrm(ks[22], (DEPTH, N_EXPERTS, D_MODEL), 0.01),
    }


def reference(x, c, ctx, c_ctx, w_mod, b_mod, w_in, pool_w, pool_scale, na_rpb, hg_lb, hg_norm_g,
              w_out, ln1_g, ln1_b, ln2_g, ln2_b, router_w, router_b, exp_w1, exp_b1, exp_w2, exp_b2):
    B, L, D = x.shape
    Lc = ctx.shape[1]
    lb_soft = jax.nn.softmax(hg_lb.astype(jnp.float32), axis=1)
    lower_bounds = jnp.cumsum(lb_soft, axis=1) - lb_soft[:, :1]
    h, hc = x, ctx
    for l in range(DEPTH):
        ctx_out = l < DEPTH - 1
        sx1, ax1, gx1, sx2, ax2, gx2 = [m[:, None] for m in adaln(c, w_mod[l], b_mod[l])]
        sc1, ac1, gc1, sc2, ac2, gc2 = adaln(c_ctx, w_mod[l], b_mod[l])
        px = modulate(layer_norm(h), sx1, ax1) @ w_in[l]
        pc = modulate(layer_norm(hc), sc1, ac1) @ w_in[l]
        mix_x, mix_c = token_mixers(px, pc, pool_w[l], pool_scale[l], na_rpb[l],
                                    lower_bounds[0, l], lower_bounds[1, l], hg_norm_g[l], ctx_out)
        h = layer_norm(DEEPNORM_ALPHA * h + gx1 * (mix_x @ w_out[l]), ln1_g[l], ln1_b[l])
        fx = modulate(layer_norm(h), sx2, ax2).reshape(B * L, D)
        moe_args = (router_w[l], router_b[l], exp_w1[l], exp_b1[l], exp_w2[l], exp_b2[l])
        if ctx_out:
            hc = layer_norm(DEEPNORM_ALPHA * hc + gc1 * (mix_c @ w_out[l]), ln1_g[l], ln1_b[l])
            fc = modulate(layer_norm(hc), sc2, ac2).reshape(B * Lc, D)
            f_all = moe_ffn(jnp.concatenate([fx, fc], axis=0), *moe_args)
            f_x = f_all[:B * L].reshape(B, L, D)
            hc = layer_norm(DEEPNORM_ALPHA * hc + gc2 * f_all[B * L:].reshape(B, Lc, D), ln2_g[l], ln2_b[l])
        else:
            f_x = moe_ffn(fx, *moe_args).reshape(B, L, D)
        h = layer_norm(DEEPNORM_ALPHA * h + gx2 * f_x, ln2_g[l], ln2_b[l])
    return h
```

```python
import numpy as np
import ml_dtypes
from contextlib import ExitStack
import concourse.bass as bass
import concourse.mybir as mybir
from concourse.bass_utils import run_bass_kernel_spmd

F32 = mybir.dt.float32
BF16 = mybir.dt.bfloat16
AF = mybir.ActivationFunctionType
ALU = mybir.AluOpType
AX = mybir.AxisListType
NPBF = ml_dtypes.bfloat16

D = 4096
KC = 32
B = 2
L = 4096
LC = 256
LT = L + LC
DEPTH = 2
NCORES = 8
GRID = 64
HD = 128
POOL_W = 1024
NA_W = 1536
HG_W = 1536
IN_W = 13312
NE = 32
DE = 512
LN_EPS = 1e-6
ALPHA = (2 * DEPTH) ** 0.25
SCALE = HD ** -0.5
NEG = -30000.0
ENGS = ("pe", "act", "dve", "pool", "sp")


class Prog:
    def __init__(self, nc, stack):
        self.nc = nc
        self.stack = stack
        self.streams = {e: [] for e in ENGS}
        self.sems = {}
        self.count = {}
        for e in ("pe", "act", "dve", "pool"):
            self.semof(e)
        self.seen = {e: {} for e in ENGS}
        self.last_write = {}
        self.readers = {}

    def semof(self, name):
        if name not in self.sems:
            self.sems[name] = self.stack.enter_context(self.nc.semaphore("s_" + name))
            self.count[name] = 0
        return self.sems[name]

    def sb(self, name, shape, dt, stack=None):
        self.uid = getattr(self, "uid", 0) + 1
        return (stack or self.stack).enter_context(self.nc.sbuf_tensor(f"sb{self.uid}_{name}", list(shape), dt))

    def ps(self, name, shape, dt, stack=None):
        self.uid = getattr(self, "uid", 0) + 1
        return (stack or self.stack).enter_context(self.nc.psum_tensor(f"pp{self.uid}_{name}", list(shape), dt))

    def _need(self, reads, writes):
        need = {}
        for k in reads:
            lw = self.last_write.get(k)
            if lw is not None:
                need[lw[0]] = max(need.get(lw[0], 0), lw[1])
        for k in writes:
            lw = self.last_write.get(k)
            if lw is not None:
                need[lw[0]] = max(need.get(lw[0], 0), lw[1])
            for (s, v) in self.readers.get(k, ()):
                need[s] = max(need.get(s, 0), v)
        return need

    def _waits(self, eng, need):
        for s, v in need.items():
            if s == "pe" and eng == "pe":
                continue
            if self.seen[eng].get(s, 0) >= v:
                continue
            self.seen[eng][s] = v
            sem = self.sems[s]
            self.streams[eng].append(lambda e, sem=sem, v=v: e.wait_ge(sem, v))

    def _commit(self, token, reads, writes):
        for k in writes:
            self.last_write[k] = token
            self.readers[k] = []
        for k in reads:
            if k in writes:
                continue
            self.readers.setdefault(k, []).append(token)

    def op(self, eng, fn, reads=(), writes=()):
        self.group(eng, [fn], reads, writes)

    def group(self, eng, fns, reads=(), writes=()):
        self._waits(eng, self._need(reads, writes))
        sem = self.sems[eng]
        if eng == "pe":
            self.count[eng] += 1
            for fn in fns[:-1]:
                self.streams[eng].append(lambda e, fn=fn: fn(e))
            fn = fns[-1]
            self.streams[eng].append(lambda e, fn=fn, sem=sem: fn(e).then_inc(sem, 1))
        else:
            for i, fn in enumerate(fns):
                if i > 0:
                    c = self.count[eng]
                    self.seen[eng][eng] = c
                    self.streams[eng].append(lambda e, sem=sem, c=c: e.wait_ge(sem, c))
                self.count[eng] += 1
                self.streams[eng].append(lambda e, fn=fn, sem=sem: fn(e).then_inc(sem, 1))
        self._commit((eng, self.count[eng]), reads, writes)

    def dma(self, eng, out, in_, reads=(), writes=(), sem=None):
        if sem is None:
            self.nuniq = getattr(self, "nuniq", 0) + 1
            sem = f"u{self.nuniq}"
        q = sem
        s = self.semof(q)
        self._waits(eng, self._need(reads, writes))
        self.count[q] += 16
        self.streams[eng].append(
            lambda e, out=out, in_=in_, s=s: e.dma_start(out=out, in_=in_).then_inc(s, 16))
        self._commit((q, self.count[q]), reads, writes)

    def barrier(self):
        for eng in ENGS:
            for s, c in self.count.items():
                if c > 0 and self.seen[eng].get(s, 0) < c and not (s == "pe" and eng == "pe"):
                    self.seen[eng][s] = c
                    sem = self.sems[s]
                    self.streams[eng].append(lambda e, sem=sem, c=c: e.wait_ge(sem, c))
        self.last_write = {}
        self.readers = {}

    def emit(self):
        for s, c in self.count.items():
            if c > 0 and self.seen["sp"].get(s, 0) < c:
                sem = self.sems[s]
                self.streams["sp"].append(lambda e, sem=sem, c=c: e.wait_ge(sem, c))
        with self.nc.Block() as block:
            @block.tensor
            def _(e):
                for f in self.streams["pe"]:
                    f(e)

            @block.scalar
            def _(e):
                for f in self.streams["act"]:
                    f(e)

            @block.vector
            def _(e):
                for f in self.streams["dve"]:
                    f(e)

            @block.gpsimd
            def _(e):
                for f in self.streams["pool"]:
                    f(e)

            @block.sync
            def _(e):
                for f in self.streams["sp"]:
                    f(e)


def new_nc():
    return bass.Bass("TRN2", target_bir_lowering=False)


def token_tiles(T):
    return [(t, min(128, T - t)) for t in range(0, T, 128)]


def token_blocks(T, bs=512):
    return [(t, min(bs, T - t)) for t in range(0, T, bs)]


def din(nc, name, shape, dt=F32):
    return nc.dram_tensor(name, list(shape), dt, kind="ExternalInput").ap()


def dout(nc, name, shape, dt=F32):
    return nc.dram_tensor(name, list(shape), dt, kind="ExternalOutput").ap()


def dscr(nc, name, shape, dt):
    return nc.dram_tensor(name, list(shape), dt).ap()


def load_w(P, wt, wk, src, nk=KC, pieces=4):
    step = max(1, nk // pieces)
    first = True
    for k0 in range(0, nk, step):
        P.dma("pool", wt[:, k0:k0 + step, :], src[:, k0:k0 + step, :], writes=[wk] if first else [], sem="L" + wk)
        first = False
    P.last_write[wk] = ("L" + wk, P.count["L" + wk])


class GemmBufs:
    def __init__(self, P, st, nk=KC, tag="g"):
        self.nk = nk
        self.wb = [P.sb(f"{tag}wb{i}", [128, nk, 512], BF16, st) for i in range(2)]
        self.xb = [P.sb(f"{tag}xb{i}", [128, nk, 512], BF16, st) for i in range(2)]
        self.ps = [P.ps(f"{tag}ps{i}", [128, 512], F32, st) for i in range(4)]
        self.tag = tag
        self.wi = 0
        self.xi = 0
        self.pi = 0


def gemm_stream(P, G, xT, xkey, T, w, N, mode, evac, x_resident=None):
    nk = G.nk
    for nb in range(N // 512):
        wt = G.wb[G.wi % 2]
        wk = f"{G.tag}wb{G.wi % 2}"
        G.wi += 1
        load_w(P, wt, wk, w[:, :, nb * 512:(nb + 1) * 512], nk)
        for (b0, bsz) in token_blocks(T):
            if x_resident is not None:
                xt, xk, xo = x_resident, xkey, b0
            else:
                xt = G.xb[G.xi % 2]
                xk = f"{G.tag}xb{G.xi % 2}"
                G.xi += 1
                xo = 0
                P.dma("sp", xt[:, :, 0:bsz], xT[:, :, b0:b0 + bsz], reads=[xkey], writes=[xk], sem="L" + xk)
            if mode == "tok":
                for (t0, tsz) in token_tiles(bsz):
                    ps = G.ps[G.pi % 4]
                    pk = f"{G.tag}ps{G.pi % 4}"
                    G.pi += 1
                    fns = [(lambda e, ps=ps, xt=xt, wt=wt, kc=kc, o=xo + t0, tsz=tsz: e.matmul(
                        ps[:tsz, :], xt[:, kc, o:o + tsz], wt[:, kc, :], start=(kc == 0), stop=(kc == nk - 1)))
                        for kc in range(nk)]
                    P.group("pe", fns, reads=[xk, wk], writes=[pk])
                    evac(ps, pk, b0 + t0, tsz, nb * 512)
            else:
                for c in range(4):
                    ps = G.ps[G.pi % 4]
                    pk = f"{G.tag}ps{G.pi % 4}"
                    G.pi += 1
                    fns = [(lambda e, ps=ps, xt=xt, wt=wt, kc=kc, c=c, xo=xo, bsz=bsz: e.matmul(
                        ps[:, :bsz], wt[:, kc, c * 128:(c + 1) * 128], xt[:, kc, xo:xo + bsz],
                        start=(kc == 0), stop=(kc == nk - 1))) for kc in range(nk)]
                    P.group("pe", fns, reads=[xk, wk], writes=[pk])
                    evac(ps, pk, nb * 512 + c * 128, b0, bsz)


class Stager:
    def __init__(self, P, st, name, shape, dt, n=4):
        self.P = P
        self.name = name
        self.t = [P.sb(f"{name}{i}", shape, dt, st) for i in range(n)]
        self.i = 0
        self.n = n

    def next(self):
        i = self.i % self.n
        self.i += 1
        return self.t[i], f"{self.name}{i}"

    def store(self, dst, src, key, dkey=None):
        self.P.dma("sp", dst, src, reads=[key], writes=[dkey] if dkey else [], sem="S" + key)


def evac_copy(P, i, out, in_, reads, writes):
    if i % 2 == 0:
        P.op("act", lambda e: e.activation(out, in_, AF.Copy), reads=reads, writes=writes)
    else:
        P.op("dve", lambda e: e.tensor_copy(out, in_), reads=reads, writes=writes)


def build_mod(NP):
    nc = new_nc()
    T = 3
    xT = din(nc, "xT", [128, KC, T])
    w = din(nc, "w", [128, KC, NP])
    bv = din(nc, "bias", [1, NP])
    out = dout(nc, "out", [T, NP])
    with ExitStack() as st:
        P = Prog(nc, st)
        G = GemmBufs(P, st)
        xf = P.sb("xf", [128, KC, T], F32)
        xs = P.sb("xs", [128, KC, T], BF16)
        bt = P.sb("bt", [T, NP], F32)
        S = Stager(P, st, "stg", [128, 512], F32)
        P.dma("sp", xf[:], xT[:, :, :], writes=["xf"])
        P.dma("sp", bt[:], bv[0:1, :].partition_broadcast(T), writes=["bt"])
        P.op("act", lambda e: e.activation(xs[:], xf[:], AF.Silu), reads=["xf"], writes=["xs"])

        def evac(ps, pk, t0, tsz, n0):
            sg, sk = S.next()
            P.op("dve", lambda e: e.tensor_tensor(sg[:tsz, :], ps[:tsz, :], bt[:tsz, n0:n0 + 512], ALU.add),
                 reads=[pk, "bt"], writes=[sk])
            S.store(out[t0:t0 + tsz, n0:n0 + 512], sg[:tsz, :], sk)

        gemm_stream(P, G, None, "xs", T, w, NP, "tok", evac, x_resident=xs)
        P.emit()
    return nc


def ln_stats(P, x, xkey, stats, mv, rstd, skey, rows=128):
    fns = []
    for c in range(8):
        fns.append(lambda e, c=c: e.bn_stats(stats[:rows, c, :], x[:rows, c * 512:(c + 1) * 512]))
    fns.append(lambda e: e.bn_aggr(mv[:rows, :], stats[:rows, :, :]))
    fns.append(lambda e: e.tensor_scalar_add(rstd[:rows, :], mv[:rows, 1:2], LN_EPS))
    P.group("dve", fns, reads=[xkey], writes=[skey])
    P.op("act", lambda e: e.activation(rstd[:rows, :], rstd[:rows, :], AF.Sqrt), reads=[skey], writes=[skey])
    P.op("dve", lambda e: e.reciprocal(rstd[:rows, :], rstd[:rows, :]), reads=[skey], writes=[skey])


def transpose_mod(P, z, zkey, ident, pts, ptbase, onep, shift, mkey, xs, xskeys, rows=128, out_f32=None):
    for g in range(8):
        pt = pts[g % len(pts)]
        pk = f"{ptbase}{g % len(pts)}"
        fns = [(lambda e, pt=pt, i=i, g=g: e.transpose(pt[:, i, :rows], z[:rows, (4 * g + i) * 128:(4 * g + i + 1) * 128],
                                                      ident[:rows, :rows])) for i in range(4)]
        P.group("pe", fns, reads=[zkey, "ident"], writes=[pk])
        fns = []
        for i in range(4):
            kc = 4 * g + i
            if g % 2 == 0:
                fns.append(lambda e, pt=pt, i=i, kc=kc: e.activation(
                    xs[:, kc, :rows], pt[:, i, :rows], AF.Identity, bias=shift[:, kc:kc + 1], scale=onep[:, kc:kc + 1]))
            else:
                fns.append(lambda e, pt=pt, i=i, kc=kc: e.tensor_scalar(
                    xs[:, kc, :rows], pt[:, i, :rows], onep[:, kc:kc + 1], shift[:, kc:kc + 1], ALU.mult, ALU.add))
        P.group("act" if g % 2 == 0 else "dve", fns, reads=[pk, mkey], writes=[xskeys[g]])


T1 = LT
NT1 = T1 // 128
CH = 32
NCH = T1 // CH
NCC = LC // CH


def build_mixer(layer, ctx_out, phases="ABCDE"):
    nc = new_nc()
    T = T1
    h_d = din(nc, "h", [T, D])
    modv_d = din(nc, "modv", [128, 4, KC])
    wtok_d = din(nc, "w_tok", [128, KC, 1024])
    wfeat_d = din(nc, "w_feat", [128, KC, 3072])
    ropec_d = din(nc, "rope_c", [128, T])
    ropes_d = din(nc, "rope_s", [128, T])
    band_d = din(nc, "band", [128, NT1, 3, 128])
    poolw_d = din(nc, "pool_w", [128, 2, 256])
    pools_d = din(nc, "pool_s", [128, 2])
    nab_d = din(nc, "na_bias", [3, 128, 8, 4, 64])
    hgv_d = din(nc, "hg_vec", [128, 3, 8])
    cmask_d = din(nc, "cmask", [64, 2, 64])
    ident_d = din(nc, "ident", [128, 128])
    mixT_d = dout(nc, "mixT", [8, 128, T], BF16)
    xnT_d = dscr(nc, "xnT", [128, KC, T], BF16)
    tokm_d = dscr(nc, "tokm", [T, 1024], BF16)
    featm_d = dscr(nc, "featm", [24, 128, T], F32)

    with ExitStack() as st:
        P = Prog(nc, st)
        ident = P.sb("ident", [128, 128], F32)
        identb = P.sb("identb", [128, 128], BF16)
        P.dma("sp", ident[:], ident_d[:, :], writes=["ident"])
        P.op("dve", lambda e: e.tensor_copy(identb[:], ident[:]), reads=["ident"], writes=["identb"])

        if "A" in phases:
            with ExitStack() as ph:
                ht = [P.sb(f"ht{i}", [128, D], F32, ph) for i in range(2)]
                xs = [P.sb(f"xs{i}", [128, KC, 128], BF16, ph) for i in range(2)]
                modv = P.sb("modv", [128, 4, KC], F32, ph)
                onep = P.sb("onep", [128, 2, KC], F32, ph)
                stats = P.sb("stats", [128, 8, 6], F32, ph)
                mv = P.sb("mv", [128, 2], F32, ph)
                rstd = P.sb("rstd", [128, 1], F32, ph)
                pts = [P.ps(f"pt{i}", [128, 4, 128], F32, ph) for i in range(4)]
                P.dma("sp", modv[:], modv_d[:, :, :], writes=["modv"])
                P.group("dve", [lambda e: e.tensor_scalar_add(onep[:, 0, :], modv[:, 0, :], 1.0),
                                lambda e: e.tensor_scalar_add(onep[:, 1, :], modv[:, 2, :], 1.0)],
                        reads=["modv"], writes=["onep"])
                for j in range(NT1):
                    x = ht[j % 2]
                    xk = f"ht{j % 2}"
                    P.dma("sp", x[:], h_d[j * 128:(j + 1) * 128, :], writes=[xk], sem="L" + xk)
                    ln_stats(P, x, xk, stats, mv, rstd, "lnst")
                    P.op("dve", lambda e, x=x: e.tensor_scalar(x[:], x[:], mv[:, 0:1], rstd[:, 0:1], ALU.subtract, ALU.mult),
                         reads=[xk, "lnst"], writes=[xk])
                    w = 1 if j < 2 else 0
                    xo = xs[j % 2]
                    keys = [f"xs{j % 2}_{g}" for g in range(8)]
                    transpose_mod(P, x, xk, ident, pts, "pt", onep[:, w, :], modv[:, 2 * w + 1, :], "onep", xo, keys)
                    P.dma("sp", xnT_d[:, :, j * 128:(j + 1) * 128], xo[:], reads=keys, writes=["xnT"], sem=f"Sxs{j % 2}")
            P.barrier()

        if "B" in phases:
            with ExitStack() as ph:
                G = GemmBufs(P, ph)
                Sb = Stager(P, ph, "sgb", [128, 512], BF16)
                Sf = Stager(P, ph, "sgf", [128, 512], F32)
                cnt = [0]

                def evac_tok(ps, pk, t0, tsz, n0):
                    sg, sk = Sb.next()
                    cnt[0] += 1
                    evac_copy(P, cnt[0], sg[:tsz, :], ps[:tsz, :], [pk], [sk])
                    Sb.store(tokm_d[t0:t0 + tsz, n0:n0 + 512], sg[:tsz, :], sk, "tokm")

                def evac_feat(ps, pk, n0, b0, bsz):
                    sg, sk = Sf.next()
                    cnt[0] += 1
                    evac_copy(P, cnt[0], sg[:, :bsz], ps[:, :bsz], [pk], [sk])
                    Sf.store(featm_d[n0 // 128, :, b0:b0 + bsz], sg[:, :bsz], sk, "featm")

                gemm_stream(P, G, xnT_d, "xnT", T, wtok_d, 1024, "tok", evac_tok)
                gemm_stream(P, G, xnT_d, "xnT", T, wfeat_d, 3072, "feat", evac_feat)
            P.barrier()

        if "C" in phases:
            with ExitStack() as ph:
                u = P.sb("u", [128, NT1, 256], BF16, ph)
                band = P.sb("band", [128, NT1, 3, 128], BF16, ph)
                dT = P.sb("dT", [128, 2, T], BF16, ph)
                pw = P.sb("pw", [128, 2, 256], BF16, ph)
                psc = P.sb("psc", [128, 2], F32, ph)
                pps = [P.ps(f"pps{i}", [128, 512], F32, ph) for i in range(4)]
                Sy = Stager(P, ph, "sgy", [128, 512], BF16)
                P.dma("sp", u[:], tokm_d[:, 0:256].rearrange("(j p) f -> p j f", p=128), writes=["u"])
                for j0 in range(0, NT1, 9):
                    j1 = min(NT1, j0 + 9)
                    P.dma("pool", band[:, j0:j1, :, :], band_d[:, j0:j1, :, :], writes=[], sem="Lband")
                P.last_write["band"] = ("Lband", P.count["Lband"])
                P.dma("pool", pw[:], poolw_d[:, :, :], writes=["pw"], sem="Lpw")
                P.dma("sp", psc[:], pools_d[:, :], writes=["psc"])
                it = 0
                for j in range(NT1):
                    lo, hi = (0, 2) if j < 2 else (2, NT1)
                    rr = [r for r in range(3) if lo <= j + r - 1 < hi]
                    for fc in range(2):
                        ps = pps[it % 4]
                        pk = f"pps{it % 4}"
                        fns = [(lambda e, ps=ps, j=j, r=r, fc=fc, first=(r == rr[0]), last=(r == rr[-1]): e.matmul(
                            ps[:, 0:128], u[:, j + r - 1, fc * 128:(fc + 1) * 128], band[:, j, r, :],
                            start=first, stop=last)) for r in rr]
                        P.group("pe", fns, reads=["u", "band"], writes=[pk])
                        evac_copy(P, it, dT[:, fc, j * 128:(j + 1) * 128], ps[:, 0:128], [pk], [f"dT{j}_{fc}"])
                        it += 1
                for (b0, bsz) in token_blocks(T):
                    dkeys = [f"dT{j}_{fc}" for j in range(b0 // 128, (b0 + bsz) // 128) for fc in range(2)]
                    for oc in range(2):
                        ps = pps[it % 4]
                        pk = f"pps{it % 4}"
                        it += 1
                        fns = [(lambda e, ps=ps, ic=ic, oc=oc, b0=b0, bsz=bsz: e.matmul(
                            ps[:, :bsz], pw[:, ic, oc * 128:(oc + 1) * 128], dT[:, ic, b0:b0 + bsz],
                            start=(ic == 0), stop=(ic == 1))) for ic in range(2)]
                        P.group("pe", fns, reads=dkeys + ["pw"], writes=[pk])
                        sg, sk = Sy.next()
                        P.op("act", lambda e, sg=sg, ps=ps, oc=oc, bsz=bsz: e.activation(
                            sg[:, :bsz], ps[:, :bsz], AF.Copy, scale=psc[:, oc:oc + 1]), reads=[pk, "psc"], writes=[sk])
                        Sy.store(mixT_d[oc, :, b0:b0 + bsz], sg[:, :bsz], sk)
            P.barrier()

        if "D" in phases:
            with ExitStack() as ph:
                fa = P.sb("fa", [128, T], F32, ph)
                fb = P.sb("fb", [128, T], F32, ph)
                rc = P.sb("rc", [128, T], F32, ph)
                rs_ = P.sb("rs", [128, T], F32, ph)
                qr = P.sb("qr", [128, T], BF16, ph)
                kr = P.sb("kr", [128, T], BF16, ph)
                Va = P.sb("Va", [128, NT1, 128], BF16, ph)
                Vb = P.sb("Vb", [128, NT1 - 1, 128], BF16, ph)
                bias = P.sb("nab", [128, 8, 4, 64], F32, ph)
                oT = P.sb("oT", [128, T], BF16, ph)
                onesb = P.sb("onesb", [128, 128], BF16, ph)
                tmp = [P.sb(f"natmp{i}", [128, 4, 64], F32, ph) for i in range(2)]
                pT = [P.sb(f"napT{i}", [128, 6, 64], BF16, ph) for i in range(2)]
                rden = [P.sb(f"rden{i}", [128, 64], F32, ph) for i in range(2)]
                pss = [P.ps(f"nps{i}", [128, 8, 64], F32, ph) for i in range(2)]
                pso = [P.ps(f"npo{i}", [128, 512], F32, ph) for i in range(2)]
                psd = [P.ps(f"npd{i}", [128, 512], F32, ph) for i in range(2)]
                P.op("pool", lambda e: e.memset(onesb[:], 1.0), writes=["onesb"])
                P.dma("sp", rc[:], ropec_d[:, :], writes=["rc"])
                P.dma("sp", rs_[:], ropes_d[:, :], writes=["rs"])
                if not ctx_out:
                    P.op("pool", lambda e: e.memset(oT[:, 0:LC], 0.0), writes=["oT"])
                for hd in range(3):
                    for (dst, dk, c0) in ((qr, "qr", 0), (kr, "kr", 2)):
                        P.dma("sp", fa[:], featm_d[4 * hd + c0, :, :], writes=["fa"], sem="Lfa")
                        P.dma("sp", fb[:], featm_d[4 * hd + c0 + 1, :, :], writes=["fb"], sem="Lfb")
                        P.op("dve", lambda e: e.tensor_tensor(fa[:], fa[:], rc[:], ALU.mult), reads=["fa", "rc"], writes=["fa"])
                        P.op("pool", lambda e: e.tensor_tensor(fb[:], fb[:], rs_[:], ALU.mult), reads=["fb", "rs"], writes=["fb"])
                        P.op("dve", lambda e, dst=dst: e.tensor_tensor(dst[:], fa[:], fb[:], ALU.add),
                             reads=["fa", "fb"], writes=[dk])
                    vcol = 256 + hd * 128
                    P.dma("sp", Va[:], tokm_d[:, vcol:vcol + 128].rearrange("(j p) f -> p j f", p=128),
                          writes=["Va"], sem="LVa")
                    P.dma("sp", Vb[:], tokm_d[64:T - 64, vcol:vcol + 128].rearrange("(j p) f -> p j f", p=128),
                          writes=["Vb"], sem="LVb")
                    P.dma("sp", bias[:], nab_d[hd, :, :, :, :], writes=["nab"], sem="Lnab")
                    blocks = []
                    if ctx_out:
                        for i in range(LC // 64):
                            blocks.append((i * 64, None, None))
                    for r in range(GRID):
                        rs0 = min(max(r - 4, 0), GRID - 8)
                        pat = r if r < 4 else (4 if r <= 60 else r - 56)
                        blocks.append((LC + 64 * r, rs0, pat))
                    for bi, (q0, rs0, pat) in enumerate(blocks):
                        i2 = bi % 2
                        chunks = [(0, Va, 0), (128, Va, 1)]
                        if rs0 is not None:
                            ks = LC + 64 * rs0
                            for c in range(4):
                                if rs0 % 2 == 0:
                                    chunks.append((ks + 128 * c, Va, (ks + 128 * c) // 128))
                                else:
                                    chunks.append((ks + 128 * c, Vb, (ks + 128 * c - 64) // 128))
                        ncnk = len(chunks)
                        ps = pss[i2]
                        fns = [(lambda e, ps=ps, c=c, k0=ch[0], q0=q0: e.matmul(
                            ps[:, c, :], kr[:, k0:k0 + 128], qr[:, q0:q0 + 64], start=True, stop=True))
                            for c, ch in enumerate(chunks)]
                        P.group("pe", fns, reads=["qr", "kr"], writes=[f"nps{i2}"])
                        p = pT[i2]
                        if rs0 is not None:
                            tm = tmp[i2]
                            P.op("dve", lambda e, tm=tm, ps=ps, pat=pat: e.scalar_tensor_tensor(
                                tm[:], ps[:, 2:6, :], SCALE, bias[:, pat, :, :], ALU.mult, ALU.add),
                                reads=[f"nps{i2}", "nab"], writes=[f"natmp{i2}"])
                            P.group("act", [
                                lambda e, p=p, ps=ps: e.activation(p[:, 0:2, :], ps[:, 0:2, :], AF.Exp, scale=SCALE),
                                lambda e, p=p, tm=tm: e.activation(p[:, 2:6, :], tm[:], AF.Exp)],
                                reads=[f"nps{i2}", f"natmp{i2}"], writes=[f"napT{i2}"])
                        else:
                            P.op("act", lambda e, p=p, ps=ps: e.activation(p[:, 0:2, :], ps[:, 0:2, :], AF.Exp, scale=SCALE),
                                 reads=[f"nps{i2}"], writes=[f"napT{i2}"])
                        po, pd = pso[i2], psd[i2]
                        fns = []
                        for c, ch in enumerate(chunks):
                            fns.append(lambda e, po=po, p=p, c=c, va=ch[1], vt=ch[2], n=ncnk: e.matmul(
                                po[:, 0:64], va[:, vt, :], p[:, c, :], start=(c == 0), stop=(c == n - 1)))
                        for c in range(ncnk):
                            fns.append(lambda e, pd=pd, p=p, c=c, n=ncnk: e.matmul(
                                pd[:, 0:64], onesb[:, :], p[:, c, :], start=(c == 0), stop=(c == n - 1)))
                        P.group("pe", fns, reads=[f"napT{i2}", "Va", "Vb", "onesb"], writes=[f"npo{i2}", f"npd{i2}"])
                        rd = rden[i2]
                        P.group("dve", [lambda e, rd=rd, pd=pd: e.reciprocal(rd[:], pd[:, 0:64]),
                                        lambda e, rd=rd, po=po, q0=q0: e.tensor_tensor(oT[:, q0:q0 + 64], po[:, 0:64], rd[:], ALU.mult)],
                                reads=[f"npo{i2}", f"npd{i2}"], writes=[f"rden{i2}", "oT"])
                    P.dma("sp", mixT_d[2 + hd, :, :], oT[:], reads=["oT"], sem="SoT")
            P.barrier()

        if "E" in phases:
            with ExitStack() as ph:
                hgv = P.sb("hgv", [128, 3, 8], F32, ph)
                lbv = P.sb("lbv", [128, 3, 2, 4], F32, ph)
                cm32 = P.sb("cm32", [CH, 2, CH], F32, ph)
                ones32 = P.sb("ones32", [128, 128], F32, ph)
                qt = [P.sb(f"qt{d}", [128, T], BF16, ph) for d in range(2)]
                kt = [P.sb(f"kt{d}", [128, T], BF16, ph) for d in range(2)]
                eend = [P.sb(f"eend{d}", [128, NCH], F32, ph) for d in range(2)]
                ema = [P.sb(f"ema{d}", [128, NCH], F32, ph) for d in range(2)]
                emb = [P.sb(f"emb{d}", [128, NCH], F32, ph) for d in range(2)]
                V64 = P.sb("V64", [CH, NCH, 128], BF16, ph)
                Sst = [P.sb(f"Sst{d}", [128, 128], F32, ph) for d in range(2)]
                Sbf = [P.sb(f"Sbf{d}", [128, 128], BF16, ph) for d in range(2)]
                P.dma("sp", hgv[:], hgv_d[:, :, :], writes=["hgv"])
                P.dma("sp", cm32[:], cmask_d[0:CH, :, 0:CH], writes=["cm32"])
                P.op("pool", lambda e: e.memset(ones32[:], 1.0 / 128.0), writes=["ones32"])
                if layer == 0:
                    P.op("pool", lambda e: e.memset(lbv[:, :, :, 0], 0.0), reads=["hgv"], writes=["lbv0"])
                else:
                    P.group("dve", [
                        lambda e: e.tensor_tensor(lbv[:, :, 0, 0], hgv[:, :, 0], hgv[:, :, 1], ALU.subtract),
                        lambda e: e.tensor_tensor(lbv[:, :, 1, 0], hgv[:, :, 2], hgv[:, :, 3], ALU.subtract)],
                        reads=["hgv"], writes=["lbv0"])
                    P.op("act", lambda e: e.activation(lbv[:, :, :, 0], lbv[:, :, :, 0], AF.Sigmoid),
                         reads=["lbv0"], writes=["lbv0"])
                P.group("dve", [
                    lambda e: e.tensor_scalar(lbv[:, :, :, 1], lbv[:, :, :, 0], -1.0, 1.0, ALU.mult, ALU.add),
                    lambda e: e.tensor_scalar(lbv[:, :, :, 2], lbv[:, :, :, 0], 1.0, -1.0, ALU.mult, ALU.add)],
                    reads=["lbv0"], writes=["lbv"])
                for hd in range(3):
                    with ExitStack() as pp:
                        qs = P.sb("hqs", [128, T], F32, pp)
                        zb = P.sb("hzb", [128, T], F32, pp)
                        sg = P.sb("hsg", [128, T], F32, pp)
                        cc = P.sb("hcc", [128, T], F32, pp)
                        e1 = P.sb("he1", [128, T], F32, pp)
                        segm = P.sb("segm", [128, NCH, CH], F32, pp)
                        P.group("pool", [lambda e: e.memset(segm[:], 1.0), lambda e: e.memset(segm[:, :, 0:1], 0.0)],
                                writes=["segm"])
                        P.dma("sp", qs[:], featm_d[12 + 4 * hd, :, :], writes=["hqs"], sem="Lhqs")
                        P.op("act", lambda e: e.activation(qs[:], qs[:], AF.Silu), reads=["hqs"], writes=["hqs"])
                        P.dma("sp", V64[:], tokm_d[:, 640 + hd * 128:640 + (hd + 1) * 128].rearrange("(c p) v -> p c v", p=CH),
                              writes=["V64"], sem="LV64")
                        for d in range(2):
                            lb = lbv[:, hd, d, 0:1]
                            oml = lbv[:, hd, d, 1:2]
                            noml = lbv[:, hd, d, 2:3]
                            P.dma("sp", zb[:], featm_d[12 + 4 * hd + 1 + d, :, :], writes=["hzb"], sem="Lhzb")
                            P.op("act", lambda e: e.activation(sg[:], zb[:], AF.Sigmoid), reads=["hzb"], writes=["hsg"])
                            P.group("dve", [
                                lambda e, oml=oml, lb=lb: e.tensor_scalar(zb[:], sg[:], oml, lb, ALU.mult, ALU.add),
                                lambda e: e.tensor_scalar_max(zb[:], zb[:], 1e-20)],
                                reads=["hsg", "lbv"], writes=["hzb"])
                            P.op("act", lambda e: e.activation(zb[:], zb[:], AF.Ln), reads=["hzb"], writes=["hzb"])
                            P.op("dve", lambda e, oml=oml, noml=noml: e.tensor_scalar(sg[:], sg[:], noml, oml, ALU.mult, ALU.add),
                                 reads=["hsg", "lbv"], writes=["hsg"])
                            P.op("dve", lambda e: e.tensor_tensor_scan(cc[:], segm[:].rearrange("p c t -> p (c t)"), zb[:], 0.0,
                                                                      ALU.mult, ALU.add),
                                 reads=["segm", "hzb"], writes=["hcc"])
                            c3 = cc[:].rearrange("p (c t) -> p c t", t=CH)
                            e3 = e1[:].rearrange("p (c t) -> p c t", t=CH)
                            MID = CH // 2
                            P.op("act", lambda e, d=d, c3=c3: e.activation(eend[d][:], c3[:, :, CH - 1], AF.Exp),
                                 reads=["hcc"], writes=[f"eend{d}"])
                            ea, eb = (ema[d], emb[d]) if d == 0 else (emb[d], ema[d])
                            P.op("act", lambda e, ea=ea, c3=c3: e.activation(ea[:], c3[:, :, MID], AF.Exp),
                                 reads=["hcc"], writes=[f"ea{d}"])
                            P.op("dve", lambda e, eb=eb, c3=c3: e.tensor_tensor(eb[:], c3[:, :, CH - 1], c3[:, :, MID], ALU.subtract),
                                 reads=["hcc"], writes=[f"eb{d}"])
                            P.op("act", lambda e, eb=eb: e.activation(eb[:], eb[:], AF.Exp), reads=[f"eb{d}"], writes=[f"eb{d}"])
                            P.op("dve", lambda e, c3=c3, e3=e3: e.tensor_tensor(e3, c3, c3[:, :, MID:MID + 1].to_broadcast([128, NCH, CH]), ALU.subtract),
                                 reads=["hcc"], writes=["he1"])
                            if d == 1:
                                P.op("dve", lambda e: e.tensor_tensor(e1[:], e1[:], zb[:], ALU.subtract),
                                     reads=["he1", "hzb"], writes=["he1"])
                            sq, sk_ = (1.0, -1.0) if d == 0 else (-1.0, 1.0)
                            P.op("act", lambda e, sq=sq: e.activation(cc[:], e1[:], AF.Exp, scale=sq),
                                 reads=["he1", f"eend{d}", f"ea{d}", f"eb{d}"], writes=["hcc"])
                            P.op("dve", lambda e, d=d: e.tensor_tensor(qt[d][:], qs[:], cc[:], ALU.mult),
                                 reads=["hqs", "hcc"], writes=[f"qt{d}"])
                            P.op("act", lambda e, sk_=sk_: e.activation(cc[:], e1[:], AF.Exp, scale=sk_),
                                 reads=["he1", f"qt{d}"], writes=["hcc"])
                            P.op("dve", lambda e, d=d: e.tensor_tensor(kt[d][:], sg[:], cc[:], ALU.mult),
                                 reads=["hsg", "hcc"], writes=[f"kt{d}"])
                    P.barrier()
                    with ExitStack() as sp_:
                        oacc = [P.sb(f"oacc{d}", [128, T], F32, sp_) for d in range(2)]
                        gbuf = P.sb("hgb", [128, T], F32, sp_)
                        At = [P.sb(f"At{d}", [CH, CH], BF16, sp_) for d in range(2)]
                        ktok = [P.sb(f"ktok{d}", [CH, 128], BF16, sp_) for d in range(2)]
                        stmp = [P.sb(f"stmp{d}", [128, 128], F32, sp_) for d in range(2)]
                        rsd = [P.sb(f"rsd{i}", [128, 512], F32, sp_) for i in range(2)]
                        Sy = Stager(P, sp_, "sgh", [128, 512], BF16)
                        psS = [P.ps(f"psS{d}", [128, 512], F32, sp_) for d in range(2)]
                        psA = [P.ps(f"psA{d}", [128, 512], F32, sp_) for d in range(2)]
                        psT = [P.ps(f"psT{d}", [128, 1024], BF16, sp_) for d in range(2)]
                        psO = [P.ps(f"psO{d}", [128, 512], F32, sp_) for d in range(2)]
                        P.dma("sp", gbuf[:], featm_d[12 + 4 * hd + 3, :, :], writes=["hgb"], sem="Lhgb")
                        P.op("act", lambda e: e.activation(gbuf[:], gbuf[:], AF.Silu), reads=["hgb"], writes=["hgb"])
                        for d in range(2):
                            P.op("pool", lambda e, d=d: e.memset(Sst[d][:], 0.0), writes=[f"Sst{d}"])
                            P.op("pool", lambda e, d=d: e.memset(Sbf[d][:], 0.0), writes=[f"Sbf{d}"])
                        order = [list(range(NCH)), list(range(NCC - 1, -1, -1)) + list(range(NCH - 1, NCC - 1, -1))]
                        for step in range(NCH):
                            for d in range(2):
                                j = order[d][step]
                                t0 = CH * j
                                ee = eend[d][:, j:j + 1]
                                ea = ema[d][:, j:j + 1]
                                eb = emb[d][:, j:j + 1]
                                P.op("act", lambda e, d=d, ea=ea: e.activation(Sbf[d][:], Sst[d][:], AF.Copy, scale=ea),
                                     reads=[f"Sst{d}", f"ema{d}"], writes=[f"Sbf{d}"])
                                P.op("pe", lambda e, d=d, t0=t0: e.matmul(psA[d][:CH, :CH], kt[d][:, t0:t0 + CH], qt[d][:, t0:t0 + CH],
                                                                          start=True, stop=True),
                                     reads=[f"kt{d}", f"qt{d}"], writes=[f"psA{d}"])
                                P.op("dve", lambda e, d=d: e.tensor_tensor(At[d][:], psA[d][:CH, :CH], cm32[:, d, :], ALU.mult),
                                     reads=[f"psA{d}", "cm32"], writes=[f"At{d}"])
                                P.op("pe", lambda e, d=d, t0=t0: e.transpose(psT[d][:CH, :128], kt[d][:, t0:t0 + CH], identb[:, :]),
                                     reads=[f"kt{d}", "identb"], writes=[f"psT{d}"])
                                P.op("act", lambda e, d=d: e.activation(ktok[d][:], psT[d][:CH, :128], AF.Copy),
                                     reads=[f"psT{d}"], writes=[f"ktok{d}"])
                                P.group("pe", [
                                    lambda e, d=d, t0=t0: e.matmul(psO[d][:, :CH], Sbf[d][:, :], qt[d][:, t0:t0 + CH], start=True, stop=False),
                                    lambda e, d=d, j=j: e.matmul(psO[d][:, :CH], V64[:, j, :], At[d][:, :], start=False, stop=True)],
                                    reads=[f"Sbf{d}", f"qt{d}", "V64", f"At{d}"], writes=[f"psO{d}"])
                                if d == 0:
                                    P.op("act", lambda e, t0=t0: e.activation(oacc[0][:, t0:t0 + CH], psO[0][:, :CH], AF.Copy),
                                         reads=["psO0"], writes=[f"oa0_{j}"])
                                else:
                                    P.op("dve", lambda e, t0=t0: e.tensor_copy(oacc[1][:, t0:t0 + CH], psO[1][:, :CH]),
                                         reads=["psO1"], writes=[f"oa1_{j}"])
                                P.op("pe", lambda e, d=d, j=j: e.matmul(psS[d][:, 0:128], ktok[d][:, :], V64[:, j, :], start=True, stop=True),
                                     reads=[f"ktok{d}", "V64"], writes=[f"psS{d}"])
                                st_ = stmp[d]
                                P.group("dve", [
                                    lambda e, d=d, eb=eb, st_=st_: e.tensor_scalar(st_[:], psS[d][:, 0:128], eb, None, ALU.mult),
                                    lambda e, d=d, ee=ee, st_=st_: e.scalar_tensor_tensor(Sst[d][:], Sst[d][:], ee, st_[:], ALU.mult, ALU.add)],
                                    reads=[f"psS{d}", f"Sst{d}", f"eend{d}", f"emb{d}", f"Sbf{d}"], writes=[f"Sst{d}", f"stmp{d}"])
                        okeys = [f"oa{d}_{j}" for d in range(2) for j in range(NCH)]
                        P.op("dve", lambda e: e.tensor_tensor(oacc[0][:], oacc[0][:], oacc[1][:], ALU.add),
                             reads=okeys, writes=["osum"])
                        P.op("act", lambda e: e.activation(oacc[1][:], oacc[0][:], AF.Square), reads=["osum"] + okeys, writes=["osq"])
                        ng = hgv[:, hd, 4:5]
                        for bi, (b0, bsz) in enumerate(token_blocks(T)):
                            pm = psS[bi % 2]
                            rs2 = rsd[bi % 2]
                            P.op("pe", lambda e, pm=pm, b0=b0, bsz=bsz: e.matmul(pm[:, :bsz], ones32[:, :], oacc[1][:, b0:b0 + bsz],
                                                                              start=True, stop=True),
                                 reads=["osq", "ones32"], writes=[f"psS{bi % 2}"])
                            P.op("dve", lambda e, pm=pm, rs2=rs2, bsz=bsz: e.tensor_scalar_add(rs2[:, :bsz], pm[:, :bsz], LN_EPS),
                                 reads=[f"psS{bi % 2}"], writes=[f"rsd{bi % 2}"])
                            P.op("act", lambda e, rs2=rs2, bsz=bsz: e.activation(rs2[:, :bsz], rs2[:, :bsz], AF.Sqrt),
                                 reads=[f"rsd{bi % 2}"], writes=[f"rsd{bi % 2}"])
                            P.op("dve", lambda e, rs2=rs2, bsz=bsz: e.reciprocal(rs2[:, :bsz], rs2[:, :bsz]),
                                 reads=[f"rsd{bi % 2}"], writes=[f"rsd{bi % 2}"])
                            rk = [f"rsd{bi % 2}"]
                            sgo, sk = Sy.next()
                            P.group("dve", [
                                lambda e, rs2=rs2, b0=b0, bsz=bsz: e.tensor_tensor(rs2[:, :bsz], rs2[:, :bsz], oacc[0][:, b0:b0 + bsz], ALU.mult),
                                lambda e, rs2=rs2, sgo=sgo, b0=b0, bsz=bsz, ng=ng: e.scalar_tensor_tensor(
                                    sgo[:, :bsz], rs2[:, :bsz], ng, gbuf[:, b0:b0 + bsz], ALU.mult, ALU.mult)],
                                reads=rk + ["osum", "hgb", "hgv"], writes=[sk] + rk)
                            Sy.store(mixT_d[5 + hd, :, b0:b0 + bsz], sgo[:, :bsz], sk)
                    P.barrier()
        P.emit()
    return nc


POOL_WINDOWS = (2, 4, 8, 16)
C_U, C_NQ, C_NK, C_NV, C_HQ, C_HF, C_HB, C_HI, C_HG = 0, 1024, 2560, 4096, 5632, 7168, 8704, 10240, 11776


def kmaj(wm):
    K, N = wm.shape
    return np.ascontiguousarray(wm.reshape(K // 128, 128, N).transpose(1, 0, 2))


def pvec(v):
    return np.ascontiguousarray(v.reshape(-1, 128).T)


def rope_perm():
    i = np.arange(128)
    half, li = i // 64, i % 64
    return half * 64 + np.where(li < 32, li + 32, li - 32)


def rope_tables():
    c = np.ones((128, LT), np.float32)
    s = np.zeros((128, LT), np.float32)
    d = np.arange(128)
    half, li = d // 64, d % 64
    inv = (10000.0 ** (-np.arange(0, 64, 2, dtype=np.float32) / 64)).astype(np.float32)
    pos = np.arange(L)
    p = np.where(half[:, None] == 0, (pos // GRID)[None, :], (pos % GRID)[None, :]).astype(np.float32)
    ang = (p * inv[li % 32][:, None]).astype(np.float32)
    c[:, LC:] = np.cos(ang)
    sn = np.sin(ang)
    s[:, LC:] = np.where((li < 32)[:, None], -sn, sn)
    return c, s


def pool_band(w):
    band = np.zeros((128, NT1, 3, 128), np.float32)
    for off, n in ((0, LC), (LC, L)):
        t = np.arange(n)
        lo = np.clip(t - w // 2, 0, n)
        hi = np.clip(t + (w - w // 2), 0, n)
        inv = (1.0 / (hi - lo).astype(np.float32)).astype(np.float32)
        for o in range(-(w // 2), w - w // 2):
            s = t + o
            ok = (s >= 0) & (s < n)
            gt, gs = off + t[ok], off + s[ok]
            np.add.at(band, (gs % 128, gt // 128, gs // 128 - gt // 128 + 1, gt % 128), inv[ok])
        gt = off + t
        np.add.at(band, (gt % 128, gt // 128, 1, gt % 128), -1.0)
    return band


def na_bias_table(rpb_h):
    out = np.full((3, 128, 8, 4, 64), NEG, np.float32)
    kk = np.arange(128)[:, None, None]
    c = np.arange(4)[None, :, None]
    qc = np.arange(64)[None, None, :]
    ko = 128 * c + kk
    krow, kcol = ko // 64, ko % 64
    c0 = np.clip(qc - 8, 0, 48)
    valid = (kcol >= c0) & (kcol < c0 + 16)
    c_off = np.clip(kcol - qc + 15, 0, 30)
    for pat in range(8):
        r = pat if pat <= 4 else 56 + pat
        rs0 = min(max(r - 4, 0), 56)
        r_off = np.broadcast_to(rs0 + krow - r + 7, valid.shape)
        for j in range(3):
            g = rpb_h[j][r_off, np.broadcast_to(c_off, valid.shape)]
            out[j, :, pat, :, :] = np.where(valid, g, NEG)
    return out


_consts = {}


def consts():
    if not _consts:
        _consts["rope"] = rope_tables()
        _consts["band"] = [pool_band(w) for w in POOL_WINDOWS]
        s = np.arange(64)[:, None]
        t = np.arange(64)[None, :]
        _consts["cmask"] = np.ascontiguousarray(np.stack([(s <= t), (s >= t)], axis=1).astype(np.float32))
        _consts["ident"] = np.eye(128, dtype=np.float32)
    return _consts


def mixer_inputs(l, hfull, mod_l, inp):
    cs = consts()
    w_in = inp["w_in"][l]
    perm = rope_perm()
    maps = []
    wq = {}
    for q in range(4):
        cols_tok = [w_in[:, C_U + 256 * q:C_U + 256 * (q + 1)]]
        cols_tok += [w_in[:, C_NV + 384 * q:C_NV + 384 * (q + 1)], w_in[:, C_HI + 384 * q:C_HI + 384 * (q + 1)]]
        feat = []
        for j in range(3):
            hh = 3 * q + j
            wq_ = w_in[:, C_NQ + 128 * hh:C_NQ + 128 * (hh + 1)]
            wk_ = w_in[:, C_NK + 128 * hh:C_NK + 128 * (hh + 1)]
            feat += [wq_, wq_[:, perm], wk_, wk_[:, perm]]
        for j in range(3):
            hh = 3 * q + j
            feat += [w_in[:, c0 + 128 * hh:c0 + 128 * (hh + 1)] for c0 in (C_HQ, C_HF, C_HB, C_HG)]
        hs = slice(384 * q, 384 * (q + 1))
        hgv = np.zeros((128, 3, 8), np.float32)
        hgv[:, :, 0] = pvec(inp["hg_lb"][0, 0, hs])
        hgv[:, :, 1] = pvec(inp["hg_lb"][0, 1, hs])
        hgv[:, :, 2] = pvec(inp["hg_lb"][1, 0, hs])
        hgv[:, :, 3] = pvec(inp["hg_lb"][1, 1, hs])
        hgv[:, :, 4] = pvec(inp["hg_norm_g"][l, hs])
        wq[q] = dict(
            w_tok=kmaj(np.concatenate(cols_tok, axis=1)), w_feat=kmaj(np.concatenate(feat, axis=1)),
            band=cs["band"][q],
            pool_w=np.ascontiguousarray(inp["pool_w"][l, q].reshape(2, 128, 256).transpose(1, 0, 2)),
            pool_s=pvec(inp["pool_scale"][l, 256 * q:256 * (q + 1)]),
            na_bias=na_bias_table(inp["na_rpb"][l, 3 * q:3 * q + 3]), hg_vec=hgv)
    for b in range(B):
        modv = np.stack([pvec(mod_l[b, D:2 * D]), pvec(mod_l[b, 0:D]), pvec(mod_l[2, D:2 * D]), pvec(mod_l[2, 0:D])], axis=1)
        for q in range(4):
            m = dict(wq[q])
            m.update(h=hfull[b], modv=np.ascontiguousarray(modv), rope_c=cs["rope"][0], rope_s=cs["rope"][1],
                     cmask=cs["cmask"], ident=cs["ident"])
            maps.append(m)
    return maps


def mix_gather(res):
    out = []
    for b in range(B):
        full = np.empty((32, 128, LT), NPBF)
        for q in range(4):
            m = res[b * 4 + q]["mixT"]
            full[2 * q:2 * q + 2] = m[0:2]
            full[8 + 3 * q:8 + 3 * q + 3] = m[2:5]
            full[20 + 3 * q:20 + 3 * q + 3] = m[5:8]
        out.append(full)
    return out


_prog_cache = {}


def run(key, builder, in_maps):
    if key not in _prog_cache:
        _prog_cache[key] = builder()
    nc = _prog_cache[key]
    res = run_bass_kernel_spmd(nc, in_maps, core_ids=list(range(NCORES)))
    return res.results


def build_post(NL, NCX, router=True):
    nc = new_nc()
    NTOK = NL + NCX
    mixT_d = din(nc, "mixT", [128, KC, NTOK], BF16)
    wout_d = din(nc, "w_out", [128, KC, D])
    h_d = din(nc, "h", [NTOK, D])
    vbc_d = din(nc, "vbc", [4, D])
    modv_d = din(nc, "modv", [128, 4, KC])
    rw_d = din(nc, "rw", [128, KC, NE])
    rb_d = din(nc, "rb", [1, NE])
    ident_d = din(nc, "ident", [128, 128])
    h1_d = dout(nc, "h1", [NTOK, D])
    fxT_d = dout(nc, "fxT", [128, KC, NTOK], BF16)
    gates_d = dout(nc, "gates", [NTOK, NE])
    y_d = dscr(nc, "ysc", [NTOK, D], F32)
    with ExitStack() as st:
        P = Prog(nc, st)
        with ExitStack() as ph:
            G = GemmBufs(P, ph)
            Sf = Stager(P, ph, "sgf", [128, 512], F32)
            cnt = [0]

            def evac(ps, pk, t0, tsz, n0):
                sg, sk = Sf.next()
                cnt[0] += 1
                evac_copy(P, cnt[0], sg[:tsz, :], ps[:tsz, :], [pk], [sk])
                Sf.store(y_d[t0:t0 + tsz, n0:n0 + 512], sg[:tsz, :], sk, "ysc")

            gemm_stream(P, G, mixT_d, "mixT", NTOK, wout_d, D, "tok", evac)
        P.barrier()
        with ExitStack() as ph:
            ident = P.sb("ident", [128, 128], F32, ph)
            vbc = P.sb("vbc", [128, 4, D], F32, ph)
            modv = P.sb("modv", [128, 4, KC], F32, ph)
            onep = P.sb("onep", [128, 2, KC], F32, ph)
            rw = P.sb("rw", [128, KC, NE], F32, ph)
            rb = P.sb("rb", [128, NE], F32, ph)
            yt = [P.sb(f"yt{i}", [128, D], F32, ph) for i in range(2)]
            hh = [P.sb(f"hh{i}", [128, D], F32, ph) for i in range(2)]
            xs32 = P.sb("xs32", [128, KC, 128], F32, ph)
            xsb = P.sb("xsb", [128, KC, 128], BF16, ph)
            stats = P.sb("stats", [128, 8, 6], F32, ph)
            mv = P.sb("mv", [128, 2], F32, ph)
            rstd = P.sb("rstd", [128, 1], F32, ph)
            lg = P.sb("lg", [128, NE], F32, ph)
            mx8 = P.sb("mx8", [128, 8], F32, ph)
            negm = P.sb("negm", [128, 1], F32, ph)
            msk = P.sb("msk", [128, NE], F32, ph)
            ex = P.sb("ex", [128, NE], F32, ph)
            ssum = P.sb("ssum", [128, 1], F32, ph)
            gt = P.sb("gt", [128, NE], F32, ph)
            pts = [P.ps(f"pt{i}", [128, 4, 128], F32, ph) for i in range(4)]
            prt = P.ps("prt", [128, 512], F32, ph)
            P.dma("sp", ident[:], ident_d[:, :], writes=["ident"])
            for i in range(4):
                P.dma("sp", vbc[:, i, :], vbc_d[i:i + 1, :].partition_broadcast(128), writes=[f"vbc{i}"])
            P.dma("sp", modv[:], modv_d[:, :, :], writes=["modv"])
            P.dma("sp", rw[:], rw_d[:, :, :], writes=["rw"])
            P.dma("sp", rb[:], rb_d[0:1, :].partition_broadcast(128), writes=["rb"])
            P.group("dve", [lambda e: e.tensor_scalar_add(onep[:, 0, :], modv[:, 0, :], 1.0),
                            lambda e: e.tensor_scalar_add(onep[:, 1, :], modv[:, 2, :], 1.0)],
                    reads=["modv"], writes=["onep"])
            tiles = [(t0, 128, 0) for t0 in range(0, NL, 128)]
            if NCX:
                tiles.append((NL, NCX, 1))
            for ti, (t0, rows, w) in enumerate(tiles):
                y, yk = yt[ti % 2], f"yt{ti % 2}"
                x, xk = hh[ti % 2], f"hh{ti % 2}"
                P.dma("sp", y[:rows, :], y_d[t0:t0 + rows, :], reads=["ysc"], writes=[yk], sem="L" + yk)
                P.dma("sp", x[:rows, :], h_d[t0:t0 + rows, :], writes=[xk], sem="L" + xk)
                P.op("pool", lambda e, y=y, rows=rows, w=w: e.tensor_tensor(y[:rows, :], y[:rows, :], vbc[:rows, w, :], ALU.mult),
                     reads=[yk, f"vbc{w}"], writes=[yk])
                P.op("dve", lambda e, y=y, x=x, rows=rows: e.scalar_tensor_tensor(x[:rows, :], x[:rows, :], ALPHA, y[:rows, :], ALU.mult, ALU.add),
                     reads=[yk, xk], writes=[xk])
                ln_stats(P, x, xk, stats, mv, rstd, "lnst", rows)
                P.op("dve", lambda e, x=x, rows=rows: e.tensor_scalar(x[:rows, :], x[:rows, :], mv[:rows, 0:1], rstd[:rows, 0:1], ALU.subtract, ALU.mult),
                     reads=[xk, "lnst"], writes=[xk])
                P.op("pool", lambda e, x=x, rows=rows: e.tensor_tensor(x[:rows, :], x[:rows, :], vbc[:rows, 2, :], ALU.mult),
                     reads=[xk, "vbc2"], writes=[xk])
                P.op("dve", lambda e, x=x, rows=rows: e.tensor_tensor(x[:rows, :], x[:rows, :], vbc[:rows, 3, :], ALU.add),
                     reads=[xk, "vbc3"], writes=[xk])
                P.dma("sp", h1_d[t0:t0 + rows, :], x[:rows, :], reads=[xk], sem="S" + xk)
                ln_stats(P, x, xk, stats, mv, rstd, "lnst", rows)
                P.op("dve", lambda e, x=x, y=y, rows=rows: e.tensor_scalar(y[:rows, :], x[:rows, :], mv[:rows, 0:1], rstd[:rows, 0:1], ALU.subtract, ALU.mult),
                     reads=[xk, "lnst"], writes=[yk])
                keys = [f"xs32_{g}" for g in range(8)]
                transpose_mod(P, y, yk, ident, pts, "pt", onep[:, w, :], modv[:, 2 * w + 1, :], "onep", xs32, keys, rows)
                P.op("pool", lambda e, rows=rows: e.tensor_copy(xsb[:, :, :rows], xs32[:, :, :rows]), reads=keys, writes=["xsb"])
                P.dma("sp", fxT_d[:, :, t0:t0 + rows], xsb[:, :, :rows], reads=["xsb"], sem="Sxsb")
                if not router:
                    continue
                fns = [(lambda e, kc=kc, rows=rows: e.matmul(prt[:rows, 0:NE], xs32[:, kc, :rows], rw[:, kc, :],
                                                            start=(kc == 0), stop=(kc == KC - 1))) for kc in range(KC)]
                P.group("pe", fns, reads=keys + ["rw"], writes=["prt"])
                fns = [lambda e, rows=rows: e.tensor_tensor(lg[:rows, :], prt[:rows, 0:NE], rb[:rows, :], ALU.add),
                       lambda e, rows=rows: e.tensor_copy(ex[:rows, :], lg[:rows, :])]
                for rnd in range(4):
                    fns.append(lambda e, rows=rows: e.tensor_reduce(mx8[:rows, 0:1], ex[:rows, :], AX.X, ALU.max))
                    if rnd == 0:
                        fns.append(lambda e, rows=rows: e.tensor_scalar(negm[:rows, :], mx8[:rows, 0:1], -1.0, None, ALU.mult))
                    if rnd < 3:
                        fns.append(lambda e, rows=rows: e.tensor_scalar(msk[:rows, :], ex[:rows, :], mx8[:rows, 0:1], None, ALU.is_ge))
                        fns.append(lambda e, rows=rows: e.scalar_tensor_tensor(ex[:rows, :], msk[:rows, :], -1e30, ex[:rows, :], ALU.mult, ALU.add))
                fns.append(lambda e, rows=rows: e.tensor_scalar(msk[:rows, :], lg[:rows, :], mx8[:rows, 0:1], None, ALU.is_ge))
                P.group("dve", fns, reads=["prt", "rb"], writes=["lg", "msk", "negm", "ex"])
                P.op("act", lambda e, rows=rows: e.activation(ex[:rows, :], lg[:rows, :], AF.Exp, bias=negm[:rows, 0:1], scale=1.0),
                     reads=["lg", "negm", "ex"], writes=["ex"])
                P.group("dve", [
                    lambda e, rows=rows: e.tensor_tensor(ex[:rows, :], ex[:rows, :], msk[:rows, :], ALU.mult),
                    lambda e, rows=rows: e.reduce_sum(ssum[:rows, :], ex[:rows, :], AX.X),
                    lambda e, rows=rows: e.reciprocal(ssum[:rows, :], ssum[:rows, :]),
                    lambda e, rows=rows: e.tensor_scalar(gt[:rows, :], ex[:rows, :], ssum[:rows, 0:1], None, ALU.mult)],
                    reads=["ex", "msk"], writes=["ex", "gt"])
                P.dma("sp", gates_d[t0:t0 + rows, :], gt[:rows, :], reads=["gt"], sem="Sgt")
        P.emit()
    return nc


def build_moe(TT):
    nc = new_nc()
    fxT_d = din(nc, "fxT", [128, KC, TT], BF16)
    gT_d = din(nc, "gT", [4, TT])
    w1_d = din(nc, "w1", [4, 128, KC, 2 * DE])
    b1_d = din(nc, "b1", [128, 4, 8])
    w2_d = din(nc, "w2", [128, 16, D])
    b2_d = din(nc, "b2", [4, D])
    out_d = dout(nc, "part", [TT, D])
    with ExitStack() as st:
        P = Prog(nc, st)
        xb = [P.sb(f"xb{i}", [128, KC, 512], BF16) for i in range(2)]
        w1b = [P.sb(f"w1b{i}", [128, KC, 2, 128], BF16) for i in range(2)]
        w2b = [P.sb(f"w2b{i}", [128, 16, 512], BF16) for i in range(2)]
        b2b = [P.sb(f"b2b{i}", [4, 512], BF16) for i in range(2)]
        gbc = [P.sb(f"gbc{i}", [128, 4, 512], F32) for i in range(2)]
        g4 = [P.sb(f"g4{i}", [4, 512], F32) for i in range(2)]
        g4b = [P.sb(f"g4b{i}", [4, 512], BF16) for i in range(2)]
        b1 = P.sb("b1", [128, 4, 8], F32)
        hd_ = [P.sb(f"hdn{i}", [128, 16, 512], BF16) for i in range(2)]
        tg = [P.sb(f"tg{i}", [128, 512], F32) for i in range(2)]
        tsg = [P.sb(f"tsg{i}", [128, 512], F32) for i in range(2)]
        tu = [P.sb(f"tu{i}", [128, 512], F32) for i in range(2)]
        S = Stager(P, st, "stg", [128, 512], F32)
        psg = [P.ps(f"psg{i}", [128, 512], F32) for i in range(2)]
        psu = [P.ps(f"psu{i}", [128, 512], F32) for i in range(2)]
        pso = [P.ps(f"pso{i}", [128, 512], F32) for i in range(4)]
        P.dma("sp", b1[:], b1_d[:, :, :], writes=["b1"])
        wi = 0
        w2i = 0
        oi = 0
        for bi, (b0, bsz) in enumerate(token_blocks(TT)):
            i2 = bi % 2
            xt, xk = xb[i2], f"xb{i2}"
            P.dma("sp", xt[:, :, :bsz], fxT_d[:, :, b0:b0 + bsz], writes=[xk], sem="L" + xk)
            for e4 in range(4):
                P.dma("sp", gbc[i2][:, e4, :bsz], gT_d[e4:e4 + 1, b0:b0 + bsz].partition_broadcast(128),
                      writes=[f"gbc{i2}"] if e4 == 0 else [], sem=f"Lgbc{i2}")
            P.last_write[f"gbc{i2}"] = (f"Lgbc{i2}", P.count[f"Lgbc{i2}"])
            P.dma("sp", g4[i2][:, :bsz], gT_d[:, b0:b0 + bsz], writes=[f"g4{i2}"], sem=f"Lg4{i2}")
            P.op("dve", lambda e, i2=i2, bsz=bsz: e.tensor_copy(g4b[i2][:, :bsz], g4[i2][:, :bsz]),
                 reads=[f"g4{i2}"], writes=[f"g4b{i2}"])
            hdn, hk = hd_[i2], f"hdn{i2}"
            for e4 in range(4):
                for j in range(4):
                    wt, wk = w1b[wi % 2], f"w1b{wi % 2}"
                    wi += 1
                    P.dma("pool", wt[:, :, 0, :], w1_d[e4, :, :, j * 128:(j + 1) * 128], writes=[wk], sem="L" + wk)
                    P.dma("pool", wt[:, :, 1, :], w1_d[e4, :, :, DE + j * 128:DE + (j + 1) * 128], writes=[], sem="L" + wk)
                    P.last_write[wk] = ("L" + wk, P.count["L" + wk])
                    k2 = (e4 * 4 + j) % 2
                    pg, pu = psg[k2], psu[k2]
                    fns = [(lambda e, pg=pg, wt=wt, xt=xt, kc=kc, bsz=bsz: e.matmul(
                        pg[:, :bsz], wt[:, kc, 0, :], xt[:, kc, :bsz], start=(kc == 0), stop=(kc == KC - 1))) for kc in range(KC)]
                    fns += [(lambda e, pu=pu, wt=wt, xt=xt, kc=kc, bsz=bsz: e.matmul(
                        pu[:, :bsz], wt[:, kc, 1, :], xt[:, kc, :bsz], start=(kc == 0), stop=(kc == KC - 1))) for kc in range(KC)]
                    P.group("pe", fns, reads=[xk, wk], writes=[f"psg{k2}", f"psu{k2}"])
                    a, s_, u = tg[k2], tsg[k2], tu[k2]
                    bg = b1[:, e4, j:j + 1]
                    bu = b1[:, e4, 4 + j:5 + j]
                    P.op("dve", lambda e, a=a, pg=pg, bg=bg, bsz=bsz: e.tensor_scalar(a[:, :bsz], pg[:, :bsz], bg, 7.0, ALU.add, ALU.min),
                         reads=[f"psg{k2}", "b1"], writes=[f"tg{k2}"])
                    P.op("act", lambda e, a=a, s_=s_, bsz=bsz: e.activation(s_[:, :bsz], a[:, :bsz], AF.Sigmoid, scale=1.702),
                         reads=[f"tg{k2}"], writes=[f"tsg{k2}"])
                    P.group("dve", [
                        lambda e, u=u, pu=pu, bu=bu, bsz=bsz: e.tensor_scalar(u[:, :bsz], pu[:, :bsz], bu, 7.0, ALU.add, ALU.min),
                        lambda e, u=u, bsz=bsz: e.tensor_scalar(u[:, :bsz], u[:, :bsz], -7.0, 1.0, ALU.max, ALU.add)],
                        reads=[f"psu{k2}", "b1"], writes=[f"tu{k2}"])
                    P.group("pool", [
                        lambda e, a=a, s_=s_, bsz=bsz: e.tensor_tensor(a[:, :bsz], a[:, :bsz], s_[:, :bsz], ALU.mult),
                        lambda e, a=a, u=u, bsz=bsz: e.tensor_tensor(a[:, :bsz], a[:, :bsz], u[:, :bsz], ALU.mult),
                        lambda e, a=a, hdn=hdn, e4=e4, j=j, i2=i2, bsz=bsz: e.tensor_tensor(
                            hdn[:, e4 * 4 + j, :bsz], a[:, :bsz], gbc[i2][:, e4, :bsz], ALU.mult)],
                        reads=[f"tg{k2}", f"tsg{k2}", f"tu{k2}", f"gbc{i2}"], writes=[f"tg{k2}", f"{hk}_{e4 * 4 + j}"])
            hkeys = [f"{hk}_{i}" for i in range(16)]
            for nb in range(8):
                wt, wk = w2b[w2i % 2], f"w2b{w2i % 2}"
                bt, bk = b2b[w2i % 2], f"b2b{w2i % 2}"
                w2i += 1
                load_w(P, wt, wk, w2_d[:, :, nb * 512:(nb + 1) * 512], 16)
                P.dma("pool", bt[:, :], b2_d[:, nb * 512:(nb + 1) * 512], writes=[bk], sem="L" + bk)
                for (t0, tsz) in token_tiles(bsz):
                    ps, pk = pso[oi % 4], f"pso{oi % 4}"
                    oi += 1
                    fns = [(lambda e, ps=ps, hdn=hdn, wt=wt, c=c, t0=t0, tsz=tsz: e.matmul(
                        ps[:tsz, :], hdn[:, c, t0:t0 + tsz], wt[:, c, :], start=(c == 0), stop=False)) for c in range(16)]
                    fns.append(lambda e, ps=ps, bt=bt, i2=i2, t0=t0, tsz=tsz: e.matmul(
                        ps[:tsz, :], g4b[i2][:, t0:t0 + tsz], bt[:, :], start=False, stop=True))
                    P.group("pe", fns, reads=hkeys + [wk, bk, f"g4b{i2}"], writes=[pk])
                    sg, sk = S.next()
                    evac_copy(P, oi, sg[:tsz, :], ps[:tsz, :], [pk], [sk])
                    S.store(out_d[b0 + t0:b0 + t0 + tsz, nb * 512:(nb + 1) * 512], sg[:tsz, :], sk)
        P.emit()
    return nc


def build_comb(NL, NCX):
    nc = new_nc()
    NTOK = NL + NCX
    part_d = din(nc, "parts", [NCORES, NTOK, D])
    h_d = din(nc, "h", [NTOK, D])
    vbc_d = din(nc, "vbc", [4, D])
    out_d = dout(nc, "h2", [NTOK, D])
    with ExitStack() as st:
        P = Prog(nc, st)
        vbc = P.sb("vbc", [128, 4, D], F32)
        acc = [P.sb(f"acc{i}", [128, D], F32) for i in range(2)]
        pt = [P.sb(f"pt{i}", [128, D], F32) for i in range(3)]
        stats = P.sb("stats", [128, 8, 6], F32)
        mv = P.sb("mv", [128, 2], F32)
        rstd = P.sb("rstd", [128, 1], F32)
        for i in range(4):
            P.dma("sp", vbc[:, i, :], vbc_d[i:i + 1, :].partition_broadcast(128), writes=[f"vbc{i}"])
        tiles = [(t0, 128, 0) for t0 in range(0, NL, 128)]
        if NCX:
            tiles.append((NL, NCX, 1))
        pi = 0
        for ti, (t0, rows, w) in enumerate(tiles):
            a, ak = acc[ti % 2], f"acc{ti % 2}"
            P.dma("sp", a[:rows, :], part_d[0, t0:t0 + rows, :], writes=[ak], sem="L" + ak)
            for c in range(1, NCORES):
                p_, pk = pt[pi % 3], f"pt{pi % 3}"
                pi += 1
                P.dma("sp", p_[:rows, :], part_d[c, t0:t0 + rows, :], writes=[pk], sem="L" + pk)
                P.op("dve" if c % 2 else "pool", lambda e, a=a, p_=p_, rows=rows: e.tensor_tensor(a[:rows, :], a[:rows, :], p_[:rows, :], ALU.add),
                     reads=[ak, pk], writes=[ak])
            p_, pk = pt[pi % 3], f"pt{pi % 3}"
            pi += 1
            P.dma("sp", p_[:rows, :], h_d[t0:t0 + rows, :], writes=[pk], sem="L" + pk)
            P.op("pool", lambda e, a=a, rows=rows, w=w: e.tensor_tensor(a[:rows, :], a[:rows, :], vbc[:rows, w, :], ALU.mult),
                 reads=[ak, f"vbc{w}"], writes=[ak])
            P.op("dve", lambda e, a=a, p_=p_, rows=rows: e.scalar_tensor_tensor(a[:rows, :], p_[:rows, :], ALPHA, a[:rows, :], ALU.mult, ALU.add),
                 reads=[ak, pk], writes=[ak])
            ln_stats(P, a, ak, stats, mv, rstd, "lnst", rows)
            P.op("dve", lambda e, a=a, rows=rows: e.tensor_scalar(a[:rows, :], a[:rows, :], mv[:rows, 0:1], rstd[:rows, 0:1], ALU.subtract, ALU.mult),
                 reads=[ak, "lnst"], writes=[ak])
            P.op("pool", lambda e, a=a, rows=rows: e.tensor_tensor(a[:rows, :], a[:rows, :], vbc[:rows, 2, :], ALU.mult),
                 reads=[ak, "vbc2"], writes=[ak])
            P.op("dve", lambda e, a=a, rows=rows: e.tensor_tensor(a[:rows, :], a[:rows, :], vbc[:rows, 3, :], ALU.add),
                 reads=[ak, "vbc3"], writes=[ak])
            P.dma("sp", out_d[t0:t0 + rows, :], a[:rows, :], reads=[ak], sem="S" + ak)
        P.emit()
    return nc


def kernel(x, c, ctx, c_ctx, w_mod, b_mod, w_in, pool_w, pool_scale, na_rpb, hg_lb, hg_norm_g,
           w_out, ln1_g, ln1_b, ln2_g, ln2_b, router_w, router_b, exp_w1, exp_b1, exp_w2, exp_b2):
    f = lambda a: np.asarray(a, dtype=np.float32)
    x, c, ctx, c_ctx, w_mod, b_mod, w_in = f(x), f(c), f(ctx), f(c_ctx), f(w_mod), f(b_mod), f(w_in)
    w_out, ln1_g, ln1_b, ln2_g, ln2_b = f(w_out), f(ln1_g), f(ln1_b), f(ln2_g), f(ln2_b)
    router_w, router_b, exp_w1, exp_b1, exp_w2, exp_b2 = f(router_w), f(router_b), f(exp_w1), f(exp_b1), f(exp_w2), f(exp_b2)
    inp = dict(w_in=w_in, pool_w=f(pool_w), pool_scale=f(pool_scale), na_rpb=f(na_rpb), hg_lb=f(hg_lb), hg_norm_g=f(hg_norm_g))
    cs = consts()
    cond = np.concatenate([c, c_ctx[None]], 0)
    condT = kmaj(np.ascontiguousarray(cond.T))
    NP = 6 * D // 4
    maps = []
    for i in range(NCORES):
        l, k = i // 4, i % 4
        maps.append(dict(xT=condT, w=kmaj(w_mod[l][:, k * NP:(k + 1) * NP]),
                         bias=np.ascontiguousarray(b_mod[l][None, k * NP:(k + 1) * NP])))
    res = run(("mod",), lambda: build_mod(NP), maps)
    mods = [np.concatenate([res[4 * l + k]["out"] for k in range(4)], axis=1) for l in range(DEPTH)]
    h = [np.array(x[b]) for b in range(B)]
    hc = [np.array(ctx[b]) for b in range(B)]
    for l in range(DEPTH):
        ctx_out = l < DEPTH - 1
        mod_l = mods[l]
        hfull = [np.concatenate([hc[b], h[b]], 0) for b in range(B)]
        res = run(("mix", l), lambda: build_mixer(l, ctx_out), mixer_inputs(l, hfull, mod_l, inp))
        mixfull = mix_gather(res)
        del res, hfull
        NL, NCX = L // 4, (LC // 4 if ctx_out else 0)
        NTOK = NL + NCX
        wo = kmaj(w_out[l])
        rwk = kmaj(router_w[l])
        maps = []
        for i in range(NCORES):
            b, qq = i // 4, i % 4
            pm = [mixfull[b][:, :, LC + NL * qq:LC + NL * (qq + 1)]]
            phh = [h[b][NL * qq:NL * (qq + 1)]]
            if ctx_out:
                pm.append(mixfull[b][:, :, NCX * qq:NCX * (qq + 1)])
                phh.append(hc[b][NCX * qq:NCX * (qq + 1)])
            mixT = np.ascontiguousarray(np.concatenate(pm, axis=2).transpose(1, 0, 2))
            vbc = np.stack([mod_l[b, 2 * D:3 * D], mod_l[2, 2 * D:3 * D], ln1_g[l], ln1_b[l]])
            modv = np.stack([pvec(mod_l[b, 4 * D:5 * D]), pvec(mod_l[b, 3 * D:4 * D]),
                             pvec(mod_l[2, 4 * D:5 * D]), pvec(mod_l[2, 3 * D:4 * D])], axis=1)
            maps.append(dict(mixT=mixT, w_out=wo, h=np.ascontiguousarray(np.concatenate(phh, 0)), vbc=np.ascontiguousarray(vbc),
                             modv=np.ascontiguousarray(modv), rw=rwk, rb=np.ascontiguousarray(router_b[l][None]), ident=cs["ident"]))
        res2 = run(("post", NL, NCX), lambda: build_post(NL, NCX), maps)
        del mixfull
        TT = NCORES * NTOK
        fxT = np.ascontiguousarray(np.concatenate([r["fxT"] for r in res2], axis=2))
        gates = np.concatenate([r["gates"] for r in res2], axis=0)
        maps = []
        for e in range(NCORES):
            es = slice(4 * e, 4 * e + 4)
            w1k = np.ascontiguousarray(exp_w1[l, es].reshape(4, KC, 128, 2 * DE).transpose(0, 2, 1, 3))
            b1k = np.ascontiguousarray(exp_b1[l, es].reshape(4, 8, 128).transpose(2, 0, 1))
            w2k = np.ascontiguousarray(exp_w2[l, es].reshape(4, 4, 128, D).transpose(2, 0, 1, 3)).reshape(128, 16, D)
            maps.append(dict(fxT=fxT, gT=np.ascontiguousarray(gates[:, es].T), w1=w1k, b1=b1k, w2=w2k,
                             b2=np.ascontiguousarray(exp_b2[l, es])))
        res3 = run(("moe", TT), lambda: build_moe(TT), maps)
        del maps, fxT
        maps = []
        for i in range(NCORES):
            b = i // 4
            parts = np.stack([r["part"][i * NTOK:(i + 1) * NTOK] for r in res3], 0)
            vbc = np.stack([mod_l[b, 5 * D:6 * D], mod_l[2, 5 * D:6 * D], ln2_g[l], ln2_b[l]])
            maps.append(dict(parts=parts, h=res2[i]["h1"], vbc=np.ascontiguousarray(vbc)))
        del res3
        res4 = run(("comb", NL, NCX), lambda: build_comb(NL, NCX), maps)
        del maps
        for i in range(NCORES):
            b, qq = i // 4, i % 4
            h2 = res4[i]["h2"]
            h[b][NL * qq:NL * (qq + 1)] = h2[:NL]
            if ctx_out:
                hc[b][NCX * qq:NCX * (qq + 1)] = h2[NL:]
    return np.stack(h).astype(np.float32)
```

```python
import numpy as np
import ml_dtypes
from contextlib import ExitStack
import concourse.bass as bass
import concourse.mybir as mybir
from concourse.bass_utils import run_bass_kernel_spmd

F32 = mybir.dt.float32
BF16 = mybir.dt.bfloat16
AF = mybir.ActivationFunctionType
ALU = mybir.AluOpType
AX = mybir.AxisListType
NPBF = ml_dtypes.bfloat16

D = 4096
KC = 32
B = 2
L = 4096
LC = 256
LT = L + LC
DEPTH = 2
NCORES = 8
GRID = 64
HD = 128
POOL_W = 1024
NA_W = 1536
HG_W = 1536
IN_W = 13312
NE = 32
DE = 512
LN_EPS = 1e-6
ALPHA = (2 * DEPTH) ** 0.25
SCALE = HD ** -0.5
NEG = -30000.0
ENGS = ("pe", "act", "dve", "pool", "sp")


class Prog:
    def __init__(self, nc, stack):
        self.nc = nc
        self.stack = stack
        self.streams = {e: [] for e in ENGS}
        self.sems = {}
        self.count = {}
        for e in ("pe", "act", "dve", "pool"):
            self.semof(e)
        self.seen = {e: {} for e in ENGS}
        self.last_write = {}
        self.readers = {}

    def semof(self, name):
        if name not in self.sems:
            self.sems[name] = self.stack.enter_context(self.nc.semaphore("s_" + name))
            self.count[name] = 0
        return self.sems[name]

    def sb(self, name, shape, dt, stack=None):
        self.uid = getattr(self, "uid", 0) + 1
        return (stack or self.stack).enter_context(self.nc.sbuf_tensor(f"sb{self.uid}_{name}", list(shape), dt))

    def ps(self, name, shape, dt, stack=None):
        self.uid = getattr(self, "uid", 0) + 1
        return (stack or self.stack).enter_context(self.nc.psum_tensor(f"pp{self.uid}_{name}", list(shape), dt))

    def _need(self, reads, writes):
        need = {}
        for k in reads:
            lw = self.last_write.get(k)
            if lw is not None:
                need[lw[0]] = max(need.get(lw[0], 0), lw[1])
        for k in writes:
            lw = self.last_write.get(k)
            if lw is not None:
                need[lw[0]] = max(need.get(lw[0], 0), lw[1])
            for (s, v) in self.readers.get(k, ()):
                need[s] = max(need.get(s, 0), v)
        return need

    def _waits(self, eng, need):
        for s, v in need.items():
            if s == "pe" and eng == "pe":
                continue
            if self.seen[eng].get(s, 0) >= v:
                continue
            self.seen[eng][s] = v
            sem = self.sems[s]
            self.streams[eng].append(lambda e, sem=sem, v=v: e.wait_ge(sem, v))

    def _commit(self, token, reads, writes):
        for k in writes:
            self.last_write[k] = token
            self.readers[k] = []
        for k in reads:
            if k in writes:
                continue
            self.readers.setdefault(k, []).append(token)

    def op(self, eng, fn, reads=(), writes=()):
        self.group(eng, [fn], reads, writes)

    def group(self, eng, fns, reads=(), writes=()):
        self._waits(eng, self._need(reads, writes))
        sem = self.sems[eng]
        if eng == "pe":
            self.count[eng] += 1
            for fn in fns[:-1]:
                self.streams[eng].append(lambda e, fn=fn: fn(e))
            fn = fns[-1]
            self.streams[eng].append(lambda e, fn=fn, sem=sem: fn(e).then_inc(sem, 1))
        else:
            for i, fn in enumerate(fns):
                if i > 0:
                    c = self.count[eng]
                    self.seen[eng][eng] = c
                    self.streams[eng].append(lambda e, sem=sem, c=c: e.wait_ge(sem, c))
                self.count[eng] += 1
                self.streams[eng].append(lambda e, fn=fn, sem=sem: fn(e).then_inc(sem, 1))
        self._commit((eng, self.count[eng]), reads, writes)

    def dma(self, eng, out, in_, reads=(), writes=(), sem=None):
        if sem is None:
            self.nuniq = getattr(self, "nuniq", 0) + 1
            sem = f"u{self.nuniq}"
        q = sem
        s = self.semof(q)
        self._waits(eng, self._need(reads, writes))
        self.count[q] += 16
        self.streams[eng].append(
            lambda e, out=out, in_=in_, s=s: e.dma_start(out=out, in_=in_).then_inc(s, 16))
        self._commit((q, self.count[q]), reads, writes)

    def barrier(self):
        for eng in ENGS:
            for s, c in self.count.items():
                if c > 0 and self.seen[eng].get(s, 0) < c and not (s == "pe" and eng == "pe"):
                    self.seen[eng][s] = c
                    sem = self.sems[s]
                    self.streams[eng].append(lambda e, sem=sem, c=c: e.wait_ge(sem, c))
        self.last_write = {}
        self.readers = {}

    def emit(self):
        for s, c in self.count.items():
            if c > 0 and self.seen["sp"].get(s, 0) < c:
                sem = self.sems[s]
                self.streams["sp"].append(lambda e, sem=sem, c=c: e.wait_ge(sem, c))
        with self.nc.Block() as block:
            @block.tensor
            def _(e):
                for f in self.streams["pe"]:
                    f(e)

            @block.scalar
            def _(e):
                for f in self.streams["act"]:
                    f(e)

            @block.vector
            def _(e):
                for f in self.streams["dve"]:
                    f(e)

            @block.gpsimd
            def _(e):
                for f in self.streams["pool"]:
                    f(e)

            @block.sync
            def _(e):
                for f in self.streams["sp"]:
                    f(e)


def new_nc():
    return bass.Bass("TRN2", target_bir_lowering=False)


def token_tiles(T):
    return [(t, min(128, T - t)) for t in range(0, T, 128)]


def token_blocks(T, bs=512):
    return [(t, min(bs, T - t)) for t in range(0, T, bs)]


def din(nc, name, shape, dt=F32):
    return nc.dram_tensor(name, list(shape), dt, kind="ExternalInput").ap()


def dout(nc, name, shape, dt=F32):
    return nc.dram_tensor(name, list(shape), dt, kind="ExternalOutput").ap()


def dscr(nc, name, shape, dt):
    return nc.dram_tensor(name, list(shape), dt).ap()


def load_w(P, wt, wk, src, nk=KC, pieces=4):
    step = max(1, nk // pieces)
    first = True
    for k0 in range(0, nk, step):
        P.dma("pool", wt[:, k0:k0 + step, :], src[:, k0:k0 + step, :], writes=[wk] if first else [], sem="L" + wk)
        first = False
    P.last_write[wk] = ("L" + wk, P.count["L" + wk])


class GemmBufs:
    def __init__(self, P, st, nk=KC, tag="g"):
        self.nk = nk
        self.wb = [P.sb(f"{tag}wb{i}", [128, nk, 512], BF16, st) for i in range(2)]
        self.xb = [P.sb(f"{tag}xb{i}", [128, nk, 512], BF16, st) for i in range(2)]
        self.ps = [P.ps(f"{tag}ps{i}", [128, 512], F32, st) for i in range(4)]
        self.tag = tag
        self.wi = 0
        self.xi = 0
        self.pi = 0


def gemm_stream(P, G, xT, xkey, T, w, N, mode, evac, x_resident=None):
    nk = G.nk
    blocks = token_blocks(T)
    steps = [(nb, b0, bsz) for nb in range(N // 512) for (b0, bsz) in blocks]

    def xload(si):
        nb, b0, bsz = steps[si]
        xt = G.xb[G.xi % 2]
        xk = f"{G.tag}xb{G.xi % 2}"
        G.xi += 1
        P.dma("sp", xt[:, :, 0:bsz], xT[:, :, b0:b0 + bsz], reads=[xkey], writes=[xk], sem="L" + xk)
        return xt, xk

    pending = None
    if x_resident is None:
        pending = xload(0)
    wt = wk = None
    for si, (nb, b0, bsz) in enumerate(steps):
        if b0 == 0:
            wt = G.wb[G.wi % 2]
            wk = f"{G.tag}wb{G.wi % 2}"
            G.wi += 1
            load_w(P, wt, wk, w[:, :, nb * 512:(nb + 1) * 512], nk)
        if x_resident is not None:
            xt, xk, xo = x_resident, xkey, b0
        else:
            xt, xk = pending
            xo = 0
            if si + 1 < len(steps):
                pending = xload(si + 1)
        if mode == "tok":
            for (t0, tsz) in token_tiles(bsz):
                ps = G.ps[G.pi % 4]
                pk = f"{G.tag}ps{G.pi % 4}"
                G.pi += 1
                fns = [(lambda e, ps=ps, xt=xt, wt=wt, kc=kc, o=xo + t0, tsz=tsz: e.matmul(
                    ps[:tsz, :], xt[:, kc, o:o + tsz], wt[:, kc, :], start=(kc == 0), stop=(kc == nk - 1)))
                    for kc in range(nk)]
                P.group("pe", fns, reads=[xk, wk], writes=[pk])
                evac(ps, pk, b0 + t0, tsz, nb * 512)
        else:
            for c in range(4):
                ps = G.ps[G.pi % 4]
                pk = f"{G.tag}ps{G.pi % 4}"
                G.pi += 1
                fns = [(lambda e, ps=ps, xt=xt, wt=wt, kc=kc, c=c, xo=xo, bsz=bsz: e.matmul(
                    ps[:, :bsz], wt[:, kc, c * 128:(c + 1) * 128], xt[:, kc, xo:xo + bsz],
                    start=(kc == 0), stop=(kc == nk - 1))) for kc in range(nk)]
                P.group("pe", fns, reads=[xk, wk], writes=[pk])
                evac(ps, pk, nb * 512 + c * 128, b0, bsz)


class Stager:
    def __init__(self, P, st, name, shape, dt, n=4):
        self.P = P
        self.name = name
        self.t = [P.sb(f"{name}{i}", shape, dt, st) for i in range(n)]
        self.i = 0
        self.n = n

    def next(self):
        i = self.i % self.n
        self.i += 1
        return self.t[i], f"{self.name}{i}"

    def store(self, dst, src, key, dkey=None):
        self.P.dma("sp", dst, src, reads=[key], writes=[dkey] if dkey else [], sem="S" + key)


def evac_copy(P, i, out, in_, reads, writes):
    if i % 2 == 0:
        P.op("act", lambda e: e.activation(out, in_, AF.Copy), reads=reads, writes=writes)
    else:
        P.op("dve", lambda e: e.tensor_copy(out, in_), reads=reads, writes=writes)


def build_mod(NP):
    nc = new_nc()
    T = 3
    xT = din(nc, "xT", [128, KC, T])
    w = din(nc, "w", [128, KC, NP])
    bv = din(nc, "bias", [1, NP])
    out = dout(nc, "out", [T, NP])
    with ExitStack() as st:
        P = Prog(nc, st)
        G = GemmBufs(P, st)
        xf = P.sb("xf", [128, KC, T], F32)
        xs = P.sb("xs", [128, KC, T], BF16)
        bt = P.sb("bt", [T, NP], F32)
        S = Stager(P, st, "stg", [128, 512], F32)
        P.dma("sp", xf[:], xT[:, :, :], writes=["xf"])
        P.dma("sp", bt[:], bv[0:1, :].partition_broadcast(T), writes=["bt"])
        P.op("act", lambda e: e.activation(xs[:], xf[:], AF.Silu), reads=["xf"], writes=["xs"])

        def evac(ps, pk, t0, tsz, n0):
            sg, sk = S.next()
            P.op("dve", lambda e: e.tensor_tensor(sg[:tsz, :], ps[:tsz, :], bt[:tsz, n0:n0 + 512], ALU.add),
                 reads=[pk, "bt"], writes=[sk])
            S.store(out[t0:t0 + tsz, n0:n0 + 512], sg[:tsz, :], sk)

        gemm_stream(P, G, None, "xs", T, w, NP, "tok", evac, x_resident=xs)
        P.emit()
    return nc


def ln_stats(P, x, xkey, stats, mv, rstd, skey, rows=128):
    fns = []
    for c in range(8):
        fns.append(lambda e, c=c: e.bn_stats(stats[:rows, c, :], x[:rows, c * 512:(c + 1) * 512]))
    fns.append(lambda e: e.bn_aggr(mv[:rows, :], stats[:rows, :, :]))
    fns.append(lambda e: e.tensor_scalar_add(rstd[:rows, :], mv[:rows, 1:2], LN_EPS))
    P.group("dve", fns, reads=[xkey], writes=[skey])
    P.op("act", lambda e: e.activation(rstd[:rows, :], rstd[:rows, :], AF.Sqrt), reads=[skey], writes=[skey])
    P.op("dve", lambda e: e.reciprocal(rstd[:rows, :], rstd[:rows, :]), reads=[skey], writes=[skey])


def transpose_mod(P, z, zkey, ident, pts, ptbase, onep, shift, mkey, xs, xskeys, rows=128, out_f32=None):
    for g in range(8):
        pt = pts[g % len(pts)]
        pk = f"{ptbase}{g % len(pts)}"
        fns = [(lambda e, pt=pt, i=i, g=g: e.transpose(pt[:, i, :rows], z[:rows, (4 * g + i) * 128:(4 * g + i + 1) * 128],
                                                      ident[:rows, :rows])) for i in range(4)]
        P.group("pe", fns, reads=[zkey, "ident"], writes=[pk])
        fns = []
        for i in range(4):
            kc = 4 * g + i
            if g % 2 == 0:
                fns.append(lambda e, pt=pt, i=i, kc=kc: e.activation(
                    xs[:, kc, :rows], pt[:, i, :rows], AF.Identity, bias=shift[:, kc:kc + 1], scale=onep[:, kc:kc + 1]))
            else:
                fns.append(lambda e, pt=pt, i=i, kc=kc: e.tensor_scalar(
                    xs[:, kc, :rows], pt[:, i, :rows], onep[:, kc:kc + 1], shift[:, kc:kc + 1], ALU.mult, ALU.add))
        P.group("act" if g % 2 == 0 else "dve", fns, reads=[pk, mkey], writes=[xskeys[g]])


T1 = LT
NT1 = T1 // 128
CH = 32
NCH = T1 // CH
NCC = LC // CH


def build_mixer(layer, ctx_out, phases="ABCDE"):
    nc = new_nc()
    T = T1
    h_d = din(nc, "h", [T, D])
    modv_d = din(nc, "modv", [128, 4, KC])
    wtok_d = din(nc, "w_tok", [128, KC, 1024])
    wfeat_d = din(nc, "w_feat", [128, KC, 3072])
    ropec_d = din(nc, "rope_c", [128, T])
    ropes_d = din(nc, "rope_s", [128, T])
    band_d = din(nc, "band", [128, NT1, 3, 128])
    poolw_d = din(nc, "pool_w", [128, 2, 256])
    pools_d = din(nc, "pool_s", [128, 2])
    nab_d = din(nc, "na_bias", [3, 128, 8, 4, 64])
    hgv_d = din(nc, "hg_vec", [128, 3, 8])
    cmask_d = din(nc, "cmask", [64, 2, 64])
    ident_d = din(nc, "ident", [128, 128])
    mixT_d = dout(nc, "mixT", [8, 128, T], BF16)
    xnT_d = dscr(nc, "xnT", [128, KC, T], BF16)
    tokm_d = dscr(nc, "tokm", [T, 1024], BF16)
    featm_d = dscr(nc, "featm", [24, 128, T], F32)

    with ExitStack() as st:
        P = Prog(nc, st)
        ident = P.sb("ident", [128, 128], F32)
        identb = P.sb("identb", [128, 128], BF16)
        P.dma("sp", ident[:], ident_d[:, :], writes=["ident"])
        P.op("dve", lambda e: e.tensor_copy(identb[:], ident[:]), reads=["ident"], writes=["identb"])

        if "A" in phases:
            with ExitStack() as ph:
                ht = [P.sb(f"ht{i}", [128, D], F32, ph) for i in range(2)]
                xs = [P.sb(f"xs{i}", [128, KC, 128], BF16, ph) for i in range(2)]
                modv = P.sb("modv", [128, 4, KC], F32, ph)
                onep = P.sb("onep", [128, 2, KC], F32, ph)
                stats = P.sb("stats", [128, 8, 6], F32, ph)
                mv = P.sb("mv", [128, 2], F32, ph)
                rstd = P.sb("rstd", [128, 1], F32, ph)
                pts = [P.ps(f"pt{i}", [128, 4, 128], F32, ph) for i in range(4)]
                P.dma("sp", modv[:], modv_d[:, :, :], writes=["modv"])
                P.group("dve", [lambda e: e.tensor_scalar_add(onep[:, 0, :], modv[:, 0, :], 1.0),
                                lambda e: e.tensor_scalar_add(onep[:, 1, :], modv[:, 2, :], 1.0)],
                        reads=["modv"], writes=["onep"])
                P.dma("sp", ht[0][:], h_d[0:128, :], writes=["ht0"], sem="Lht0")
                for j in range(NT1):
                    x = ht[j % 2]
                    xk = f"ht{j % 2}"
                    if j + 1 < NT1:
                        P.dma("sp", ht[(j + 1) % 2][:], h_d[(j + 1) * 128:(j + 2) * 128, :], writes=[f"ht{(j + 1) % 2}"],
                              sem=f"Lht{(j + 1) % 2}")
                    ln_stats(P, x, xk, stats, mv, rstd, "lnst")
                    P.op("dve", lambda e, x=x: e.tensor_scalar(x[:], x[:], mv[:, 0:1], rstd[:, 0:1], ALU.subtract, ALU.mult),
                         reads=[xk, "lnst"], writes=[xk])
                    w = 1 if j < 2 else 0
                    xo = xs[j % 2]
                    keys = [f"xs{j % 2}_{g}" for g in range(8)]
                    transpose_mod(P, x, xk, ident, pts, "pt", onep[:, w, :], modv[:, 2 * w + 1, :], "onep", xo, keys)
                    P.dma("sp", xnT_d[:, :, j * 128:(j + 1) * 128], xo[:], reads=keys, writes=["xnT"], sem=f"Sxs{j % 2}")
            P.barrier()

        if "B" in phases:
            with ExitStack() as ph:
                G = GemmBufs(P, ph)
                Sb = Stager(P, ph, "sgb", [128, 512], BF16)
                Sf = Stager(P, ph, "sgf", [128, 512], F32)
                cnt = [0]

                def evac_tok(ps, pk, t0, tsz, n0):
                    sg, sk = Sb.next()
                    cnt[0] += 1
                    evac_copy(P, cnt[0], sg[:tsz, :], ps[:tsz, :], [pk], [sk])
                    Sb.store(tokm_d[t0:t0 + tsz, n0:n0 + 512], sg[:tsz, :], sk, "tokm")

                def evac_feat(ps, pk, n0, b0, bsz):
                    sg, sk = Sf.next()
                    cnt[0] += 1
                    evac_copy(P, cnt[0], sg[:, :bsz], ps[:, :bsz], [pk], [sk])
                    Sf.store(featm_d[n0 // 128, :, b0:b0 + bsz], sg[:, :bsz], sk, "featm")

                gemm_stream(P, G, xnT_d, "xnT", T, wtok_d, 1024, "tok", evac_tok)
                gemm_stream(P, G, xnT_d, "xnT", T, wfeat_d, 3072, "feat", evac_feat)
            P.barrier()

        if "C" in phases:
            with ExitStack() as ph:
                u = P.sb("u", [128, NT1, 256], BF16, ph)
                band = P.sb("band", [128, NT1, 3, 128], BF16, ph)
                dT = P.sb("dT", [128, 2, T], BF16, ph)
                pw = P.sb("pw", [128, 2, 256], BF16, ph)
                psc = P.sb("psc", [128, 2], F32, ph)
                pps = [P.ps(f"pps{i}", [128, 512], F32, ph) for i in range(4)]
                Sy = Stager(P, ph, "sgy", [128, 512], BF16)
                P.dma("sp", u[:], tokm_d[:, 0:256].rearrange("(j p) f -> p j f", p=128), writes=["u"])
                for j0 in range(0, NT1, 9):
                    j1 = min(NT1, j0 + 9)
                    P.dma("pool", band[:, j0:j1, :, :], band_d[:, j0:j1, :, :], writes=[], sem="Lband")
                P.last_write["band"] = ("Lband", P.count["Lband"])
                P.dma("pool", pw[:], poolw_d[:, :, :], writes=["pw"], sem="Lpw")
                P.dma("sp", psc[:], pools_d[:, :], writes=["psc"])
                it = 0
                for j in range(NT1):
                    lo, hi = (0, 2) if j < 2 else (2, NT1)
                    rr = [r for r in range(3) if lo <= j + r - 1 < hi]
                    for fc in range(2):
                        ps = pps[it % 4]
                        pk = f"pps{it % 4}"
                        fns = [(lambda e, ps=ps, j=j, r=r, fc=fc, first=(r == rr[0]), last=(r == rr[-1]): e.matmul(
                            ps[:, 0:128], u[:, j + r - 1, fc * 128:(fc + 1) * 128], band[:, j, r, :],
                            start=first, stop=last)) for r in rr]
                        P.group("pe", fns, reads=["u", "band"], writes=[pk])
                        evac_copy(P, it, dT[:, fc, j * 128:(j + 1) * 128], ps[:, 0:128], [pk], [f"dT{j}_{fc}"])
                        it += 1
                for (b0, bsz) in token_blocks(T):
                    dkeys = [f"dT{j}_{fc}" for j in range(b0 // 128, (b0 + bsz) // 128) for fc in range(2)]
                    for oc in range(2):
                        ps = pps[it % 4]
                        pk = f"pps{it % 4}"
                        it += 1
                        fns = [(lambda e, ps=ps, ic=ic, oc=oc, b0=b0, bsz=bsz: e.matmul(
                            ps[:, :bsz], pw[:, ic, oc * 128:(oc + 1) * 128], dT[:, ic, b0:b0 + bsz],
                            start=(ic == 0), stop=(ic == 1))) for ic in range(2)]
                        P.group("pe", fns, reads=dkeys + ["pw"], writes=[pk])
                        sg, sk = Sy.next()
                        P.op("act", lambda e, sg=sg, ps=ps, oc=oc, bsz=bsz: e.activation(
                            sg[:, :bsz], ps[:, :bsz], AF.Copy, scale=psc[:, oc:oc + 1]), reads=[pk, "psc"], writes=[sk])
                        Sy.store(mixT_d[oc, :, b0:b0 + bsz], sg[:, :bsz], sk)
            P.barrier()

        if "D" in phases:
            with ExitStack() as ph:
                fa = P.sb("fa", [128, T], F32, ph)
                fb = P.sb("fb", [128, T], F32, ph)
                rc = P.sb("rc", [128, T], F32, ph)
                rs_ = P.sb("rs", [128, T], F32, ph)
                qr = P.sb("qr", [128, T], BF16, ph)
                kr = P.sb("kr", [128, T], BF16, ph)
                Va = P.sb("Va", [128, NT1, 128], BF16, ph)
                Vb = P.sb("Vb", [128, NT1 - 1, 128], BF16, ph)
                bias = P.sb("nab", [128, 8, 4, 64], F32, ph)
                oT = P.sb("oT", [128, T], BF16, ph)
                onesb = P.sb("onesb", [128, 128], BF16, ph)
                tmp = [P.sb(f"natmp{i}", [128, 4, 64], F32, ph) for i in range(2)]
                pT = [P.sb(f"napT{i}", [128, 6, 64], BF16, ph) for i in range(2)]
                rden = [P.sb(f"rden{i}", [128, 64], F32, ph) for i in range(2)]
                pss = [P.ps(f"nps{i}", [128, 8, 64], F32, ph) for i in range(2)]
                pso = [P.ps(f"npo{i}", [128, 512], F32, ph) for i in range(2)]
                psd = [P.ps(f"npd{i}", [128, 512], F32, ph) for i in range(2)]
                P.op("pool", lambda e: e.memset(onesb[:], 1.0), writes=["onesb"])
                P.dma("sp", rc[:], ropec_d[:, :], writes=["rc"])
                P.dma("sp", rs_[:], ropes_d[:, :], writes=["rs"])
                if not ctx_out:
                    P.op("pool", lambda e: e.memset(oT[:, 0:LC], 0.0), writes=["oT"])
                for hd in range(3):
                    for (dst, dk, c0) in ((qr, "qr", 0), (kr, "kr", 2)):
                        P.dma("sp", fa[:], featm_d[4 * hd + c0, :, :], writes=["fa"], sem="Lfa")
                        P.dma("sp", fb[:], featm_d[4 * hd + c0 + 1, :, :], writes=["fb"], sem="Lfb")
                        P.op("dve", lambda e: e.tensor_tensor(fa[:], fa[:], rc[:], ALU.mult), reads=["fa", "rc"], writes=["fa"])
                        P.op("pool", lambda e: e.tensor_tensor(fb[:], fb[:], rs_[:], ALU.mult), reads=["fb", "rs"], writes=["fb"])
                        P.op("dve", lambda e, dst=dst: e.tensor_tensor(dst[:], fa[:], fb[:], ALU.add),
                             reads=["fa", "fb"], writes=[dk])
                    vcol = 256 + hd * 128
                    P.dma("sp", Va[:], tokm_d[:, vcol:vcol + 128].rearrange("(j p) f -> p j f", p=128),
                          writes=["Va"], sem="LVa")
                    P.dma("sp", Vb[:], tokm_d[64:T - 64, vcol:vcol + 128].rearrange("(j p) f -> p j f", p=128),
                          writes=["Vb"], sem="LVb")
                    P.dma("sp", bias[:], nab_d[hd, :, :, :, :], writes=["nab"], sem="Lnab")
                    blocks = []
                    if ctx_out:
                        for i in range(LC // 64):
                            blocks.append((i * 64, None, None))
                    for r in range(GRID):
                        rs0 = min(max(r - 4, 0), GRID - 8)
                        pat = r if r < 4 else (4 if r <= 60 else r - 56)
                        blocks.append((LC + 64 * r, rs0, pat))
                    for bi, (q0, rs0, pat) in enumerate(blocks):
                        i2 = bi % 2
                        chunks = [(0, Va, 0), (128, Va, 1)]
                        if rs0 is not None:
                            ks = LC + 64 * rs0
                            for c in range(4):
                                if rs0 % 2 == 0:
                                    chunks.append((ks + 128 * c, Va, (ks + 128 * c) // 128))
                                else:
                                    chunks.append((ks + 128 * c, Vb, (ks + 128 * c - 64) // 128))
                        ncnk = len(chunks)
                        ps = pss[i2]
                        fns = [(lambda e, ps=ps, c=c, k0=ch[0], q0=q0: e.matmul(
                            ps[:, c, :], kr[:, k0:k0 + 128], qr[:, q0:q0 + 64], start=True, stop=True))
                            for c, ch in enumerate(chunks)]
                        P.group("pe", fns, reads=["qr", "kr"], writes=[f"nps{i2}"])
                        p = pT[i2]
                        if rs0 is not None:
                            tm = tmp[i2]
                            P.op("dve", lambda e, tm=tm, ps=ps, pat=pat: e.scalar_tensor_tensor(
                                tm[:], ps[:, 2:6, :], SCALE, bias[:, pat, :, :], ALU.mult, ALU.add),
                                reads=[f"nps{i2}", "nab"], writes=[f"natmp{i2}"])
                            P.group("act", [
                                lambda e, p=p, ps=ps: e.activation(p[:, 0:2, :], ps[:, 0:2, :], AF.Exp, scale=SCALE),
                                lambda e, p=p, tm=tm: e.activation(p[:, 2:6, :], tm[:], AF.Exp)],
                                reads=[f"nps{i2}", f"natmp{i2}"], writes=[f"napT{i2}"])
                        else:
                            P.op("act", lambda e, p=p, ps=ps: e.activation(p[:, 0:2, :], ps[:, 0:2, :], AF.Exp, scale=SCALE),
                                 reads=[f"nps{i2}"], writes=[f"napT{i2}"])
                        po, pd = pso[i2], psd[i2]
                        fns = []
                        for c, ch in enumerate(chunks):
                            fns.append(lambda e, po=po, p=p, c=c, va=ch[1], vt=ch[2], n=ncnk: e.matmul(
                                po[:, 0:64], va[:, vt, :], p[:, c, :], start=(c == 0), stop=(c == n - 1)))
                        for c in range(ncnk):
                            fns.append(lambda e, pd=pd, p=p, c=c, n=ncnk: e.matmul(
                                pd[:, 0:64], onesb[:, :], p[:, c, :], start=(c == 0), stop=(c == n - 1)))
                        P.group("pe", fns, reads=[f"napT{i2}", "Va", "Vb", "onesb"], writes=[f"npo{i2}", f"npd{i2}"])
                        rd = rden[i2]
                        P.group("dve", [lambda e, rd=rd, pd=pd: e.reciprocal(rd[:], pd[:, 0:64]),
                                        lambda e, rd=rd, po=po, q0=q0: e.tensor_tensor(oT[:, q0:q0 + 64], po[:, 0:64], rd[:], ALU.mult)],
                                reads=[f"npo{i2}", f"npd{i2}"], writes=[f"rden{i2}", "oT"])
                    P.dma("sp", mixT_d[2 + hd, :, :], oT[:], reads=["oT"], sem="SoT")
            P.barrier()

        if "E" in phases:
            with ExitStack() as ph:
                hgv = P.sb("hgv", [128, 3, 8], F32, ph)
                lbv = P.sb("lbv", [128, 3, 2, 4], F32, ph)
                cm32 = P.sb("cm32", [CH, 2, CH], F32, ph)
                ones32 = P.sb("ones32", [128, 128], F32, ph)
                qt = [P.sb(f"qt{d}", [128, T], BF16, ph) for d in range(2)]
                kt = [P.sb(f"kt{d}", [128, T], BF16, ph) for d in range(2)]
                eend = [P.sb(f"eend{d}", [128, NCH], F32, ph) for d in range(2)]
                ema = [P.sb(f"ema{d}", [128, NCH], F32, ph) for d in range(2)]
                emb = [P.sb(f"emb{d}", [128, NCH], F32, ph) for d in range(2)]
                V64 = P.sb("V64", [CH, NCH, 128], BF16, ph)
                Sst = [P.sb(f"Sst{d}", [128, 128], F32, ph) for d in range(2)]
                Sbf = [P.sb(f"Sbf{d}", [128, 128], BF16, ph) for d in range(2)]
                P.dma("sp", hgv[:], hgv_d[:, :, :], writes=["hgv"])
                P.dma("sp", cm32[:], cmask_d[0:CH, :, 0:CH], writes=["cm32"])
                P.op("pool", lambda e: e.memset(ones32[:], 1.0 / 128.0), writes=["ones32"])
                if layer == 0:
                    P.op("pool", lambda e: e.memset(lbv[:, :, :, 0], 0.0), reads=["hgv"], writes=["lbv0"])
                else:
                    P.group("dve", [
                        lambda e: e.tensor_tensor(lbv[:, :, 0, 0], hgv[:, :, 0], hgv[:, :, 1], ALU.subtract),
                        lambda e: e.tensor_tensor(lbv[:, :, 1, 0], hgv[:, :, 2], hgv[:, :, 3], ALU.subtract)],
                        reads=["hgv"], writes=["lbv0"])
                    P.op("act", lambda e: e.activation(lbv[:, :, :, 0], lbv[:, :, :, 0], AF.Sigmoid),
                         reads=["lbv0"], writes=["lbv0"])
                P.group("dve", [
                    lambda e: e.tensor_scalar(lbv[:, :, :, 1], lbv[:, :, :, 0], -1.0, 1.0, ALU.mult, ALU.add),
                    lambda e: e.tensor_scalar(lbv[:, :, :, 2], lbv[:, :, :, 0], 1.0, -1.0, ALU.mult, ALU.add)],
                    reads=["lbv0"], writes=["lbv"])
                for hd in range(3):
                    with ExitStack() as pp:
                        qs = P.sb("hqs", [128, T], F32, pp)
                        zb = P.sb("hzb", [128, T], F32, pp)
                        sg = P.sb("hsg", [128, T], F32, pp)
                        cc = P.sb("hcc", [128, T], F32, pp)
                        e1 = P.sb("he1", [128, T], F32, pp)
                        segm = P.sb("segm", [128, NCH, CH], F32, pp)
                        P.group("pool", [lambda e: e.memset(segm[:], 1.0), lambda e: e.memset(segm[:, :, 0:1], 0.0)],
                                writes=["segm"])
                        P.dma("sp", qs[:], featm_d[12 + 4 * hd, :, :], writes=["hqs"], sem="Lhqs")
                        P.op("act", lambda e: e.activation(qs[:], qs[:], AF.Silu), reads=["hqs"], writes=["hqs"])
                        P.dma("sp", V64[:], tokm_d[:, 640 + hd * 128:640 + (hd + 1) * 128].rearrange("(c p) v -> p c v", p=CH),
                              writes=["V64"], sem="LV64")
                        for d in range(2):
                            lb = lbv[:, hd, d, 0:1]
                            oml = lbv[:, hd, d, 1:2]
                            noml = lbv[:, hd, d, 2:3]
                            P.dma("sp", zb[:], featm_d[12 + 4 * hd + 1 + d, :, :], writes=["hzb"], sem="Lhzb")
                            P.op("act", lambda e: e.activation(sg[:], zb[:], AF.Sigmoid), reads=["hzb"], writes=["hsg"])
                            P.group("dve", [
                                lambda e, oml=oml, lb=lb: e.tensor_scalar(zb[:], sg[:], oml, lb, ALU.mult, ALU.add),
                                lambda e: e.tensor_scalar_max(zb[:], zb[:], 1e-20)],
                                reads=["hsg", "lbv"], writes=["hzb"])
                            P.op("act", lambda e: e.activation(zb[:], zb[:], AF.Ln), reads=["hzb"], writes=["hzb"])
                            P.op("dve", lambda e, oml=oml, noml=noml: e.tensor_scalar(sg[:], sg[:], noml, oml, ALU.mult, ALU.add),
                                 reads=["hsg", "lbv"], writes=["hsg"])
                            P.op("dve", lambda e: e.tensor_tensor_scan(cc[:], segm[:].rearrange("p c t -> p (c t)"), zb[:], 0.0,
                                                                      ALU.mult, ALU.add),
                                 reads=["segm", "hzb"], writes=["hcc"])
                            c3 = cc[:].rearrange("p (c t) -> p c t", t=CH)
                            e3 = e1[:].rearrange("p (c t) -> p c t", t=CH)
                            MID = CH // 2
                            P.op("act", lambda e, d=d, c3=c3: e.activation(eend[d][:], c3[:, :, CH - 1], AF.Exp),
                                 reads=["hcc"], writes=[f"eend{d}"])
                            ea, eb = (ema[d], emb[d]) if d == 0 else (emb[d], ema[d])
                            P.op("act", lambda e, ea=ea, c3=c3: e.activation(ea[:], c3[:, :, MID], AF.Exp),
                                 reads=["hcc"], writes=[f"ea{d}"])
                            P.op("dve", lambda e, eb=eb, c3=c3: e.tensor_tensor(eb[:], c3[:, :, CH - 1], c3[:, :, MID], ALU.subtract),
                                 reads=["hcc"], writes=[f"eb{d}"])
                            P.op("act", lambda e, eb=eb: e.activation(eb[:], eb[:], AF.Exp), reads=[f"eb{d}"], writes=[f"eb{d}"])
                            P.op("dve", lambda e, c3=c3, e3=e3: e.tensor_tensor(e3, c3, c3[:, :, MID:MID + 1].to_broadcast([128, NCH, CH]), ALU.subtract),
                                 reads=["hcc"], writes=["he1"])
                            if d == 1:
                                P.op("dve", lambda e: e.tensor_tensor(e1[:], e1[:], zb[:], ALU.subtract),
                                     reads=["he1", "hzb"], writes=["he1"])
                            sq, sk_ = (1.0, -1.0) if d == 0 else (-1.0, 1.0)
                            P.op("act", lambda e, sq=sq: e.activation(cc[:], e1[:], AF.Exp, scale=sq),
                                 reads=["he1", f"eend{d}", f"ea{d}", f"eb{d}"], writes=["hcc"])
                            P.op("dve", lambda e, d=d: e.tensor_tensor(qt[d][:], qs[:], cc[:], ALU.mult),
                                 reads=["hqs", "hcc"], writes=[f"qt{d}"])
                            P.op("act", lambda e, sk_=sk_: e.activation(cc[:], e1[:], AF.Exp, scale=sk_),
                                 reads=["he1", f"qt{d}"], writes=["hcc"])
                            P.op("dve", lambda e, d=d: e.tensor_tensor(kt[d][:], sg[:], cc[:], ALU.mult),
                                 reads=["hsg", "hcc"], writes=[f"kt{d}"])
                    P.barrier()
                    with ExitStack() as sp_:
                        oacc = [P.sb(f"oacc{d}", [128, T], F32, sp_) for d in range(2)]
                        gbuf = P.sb("hgb", [128, T], F32, sp_)
                        At = [P.sb(f"At{d}", [CH, CH], BF16, sp_) for d in range(2)]
                        ktok = [P.sb(f"ktok{d}", [CH, 128], BF16, sp_) for d in range(2)]
                        stmp = [P.sb(f"stmp{d}", [128, 128], F32, sp_) for d in range(2)]
                        rsd = [P.sb(f"rsd{i}", [128, 512], F32, sp_) for i in range(2)]
                        Sy = Stager(P, sp_, "sgh", [128, 512], BF16)
                        psS = [P.ps(f"psS{d}", [128, 512], F32, sp_) for d in range(2)]
                        psA = [P.ps(f"psA{d}", [128, 512], F32, sp_) for d in range(2)]
                        psT = [P.ps(f"psT{d}", [128, 1024], BF16, sp_) for d in range(2)]
                        psO = [P.ps(f"psO{d}", [128, 512], F32, sp_) for d in range(2)]
                        P.dma("sp", gbuf[:], featm_d[12 + 4 * hd + 3, :, :], writes=["hgb"], sem="Lhgb")
                        P.op("act", lambda e: e.activation(gbuf[:], gbuf[:], AF.Silu), reads=["hgb"], writes=["hgb"])
                        for d in range(2):
                            P.op("pool", lambda e, d=d: e.memset(Sst[d][:], 0.0), writes=[f"Sst{d}"])
                            P.op("pool", lambda e, d=d: e.memset(Sbf[d][:], 0.0), writes=[f"Sbf{d}"])
                        order = [list(range(NCH)), list(range(NCC - 1, -1, -1)) + list(range(NCH - 1, NCC - 1, -1))]
                        for step in range(NCH):
                            for d in range(2):
                                j = order[d][step]
                                t0 = CH * j
                                ee = eend[d][:, j:j + 1]
                                ea = ema[d][:, j:j + 1]
                                eb = emb[d][:, j:j + 1]
                                P.op("act", lambda e, d=d, ea=ea: e.activation(Sbf[d][:], Sst[d][:], AF.Copy, scale=ea),
                                     reads=[f"Sst{d}", f"ema{d}"], writes=[f"Sbf{d}"])
                                P.op("pe", lambda e, d=d, t0=t0: e.matmul(psA[d][:CH, :CH], kt[d][:, t0:t0 + CH], qt[d][:, t0:t0 + CH],
                                                                          start=True, stop=True),
                                     reads=[f"kt{d}", f"qt{d}"], writes=[f"psA{d}"])
                                P.op("dve", lambda e, d=d: e.tensor_tensor(At[d][:], psA[d][:CH, :CH], cm32[:, d, :], ALU.mult),
                                     reads=[f"psA{d}", "cm32"], writes=[f"At{d}"])
                                P.op("pe", lambda e, d=d, t0=t0: e.transpose(psT[d][:CH, :128], kt[d][:, t0:t0 + CH], identb[:, :]),
                                     reads=[f"kt{d}", "identb"], writes=[f"psT{d}"])
                                P.op("act", lambda e, d=d: e.activation(ktok[d][:], psT[d][:CH, :128], AF.Copy),
                                     reads=[f"psT{d}"], writes=[f"ktok{d}"])
                                P.group("pe", [
                                    lambda e, d=d, t0=t0: e.matmul(psO[d][:, :CH], Sbf[d][:, :], qt[d][:, t0:t0 + CH], start=True, stop=False),
                                    lambda e, d=d, j=j: e.matmul(psO[d][:, :CH], V64[:, j, :], At[d][:, :], start=False, stop=True)],
                                    reads=[f"Sbf{d}", f"qt{d}", "V64", f"At{d}"], writes=[f"psO{d}"])
                                if d == 0:
                                    P.op("act", lambda e, t0=t0: e.activation(oacc[0][:, t0:t0 + CH], psO[0][:, :CH], AF.Copy),
                                         reads=["psO0"], writes=[f"oa0_{j}"])
                                else:
                                    P.op("dve", lambda e, t0=t0: e.tensor_copy(oacc[1][:, t0:t0 + CH], psO[1][:, :CH]),
                                         reads=["psO1"], writes=[f"oa1_{j}"])
                                P.op("pe", lambda e, d=d, j=j: e.matmul(psS[d][:, 0:128], ktok[d][:, :], V64[:, j, :], start=True, stop=True),
                                     reads=[f"ktok{d}", "V64"], writes=[f"psS{d}"])
                                st_ = stmp[d]
                                P.group("dve", [
                                    lambda e, d=d, eb=eb, st_=st_: e.tensor_scalar(st_[:], psS[d][:, 0:128], eb, None, ALU.mult),
                                    lambda e, d=d, ee=ee, st_=st_: e.scalar_tensor_tensor(Sst[d][:], Sst[d][:], ee, st_[:], ALU.mult, ALU.add)],
                                    reads=[f"psS{d}", f"Sst{d}", f"eend{d}", f"emb{d}", f"Sbf{d}"], writes=[f"Sst{d}", f"stmp{d}"])
                        okeys = [f"oa{d}_{j}" for d in range(2) for j in range(NCH)]
                        P.op("dve", lambda e: e.tensor_tensor(oacc[0][:], oacc[0][:], oacc[1][:], ALU.add),
                             reads=okeys, writes=["osum"])
                        P.op("act", lambda e: e.activation(oacc[1][:], oacc[0][:], AF.Square), reads=["osum"] + okeys, writes=["osq"])
                        ng = hgv[:, hd, 4:5]
                        for bi, (b0, bsz) in enumerate(token_blocks(T)):
                            pm = psS[bi % 2]
                            rs2 = rsd[bi % 2]
                            P.op("pe", lambda e, pm=pm, b0=b0, bsz=bsz: e.matmul(pm[:, :bsz], ones32[:, :], oacc[1][:, b0:b0 + bsz],
                                                                              start=True, stop=True),
                                 reads=["osq", "ones32"], writes=[f"psS{bi % 2}"])
                            P.op("dve", lambda e, pm=pm, rs2=rs2, bsz=bsz: e.tensor_scalar_add(rs2[:, :bsz], pm[:, :bsz], LN_EPS),
                                 reads=[f"psS{bi % 2}"], writes=[f"rsd{bi % 2}"])
                            P.op("act", lambda e, rs2=rs2, bsz=bsz: e.activation(rs2[:, :bsz], rs2[:, :bsz], AF.Sqrt),
                                 reads=[f"rsd{bi % 2}"], writes=[f"rsd{bi % 2}"])
                            P.op("dve", lambda e, rs2=rs2, bsz=bsz: e.reciprocal(rs2[:, :bsz], rs2[:, :bsz]),
                                 reads=[f"rsd{bi % 2}"], writes=[f"rsd{bi % 2}"])
                            rk = [f"rsd{bi % 2}"]
                            sgo, sk = Sy.next()
                            P.group("dve", [
                                lambda e, rs2=rs2, b0=b0, bsz=bsz: e.tensor_tensor(rs2[:, :bsz], rs2[:, :bsz], oacc[0][:, b0:b0 + bsz], ALU.mult),
                                lambda e, rs2=rs2, sgo=sgo, b0=b0, bsz=bsz, ng=ng: e.scalar_tensor_tensor(
                                    sgo[:, :bsz], rs2[:, :bsz], ng, gbuf[:, b0:b0 + bsz], ALU.mult, ALU.mult)],
                                reads=rk + ["osum", "hgb", "hgv"], writes=[sk] + rk)
                            Sy.store(mixT_d[5 + hd, :, b0:b0 + bsz], sgo[:, :bsz], sk)
                    P.barrier()
        P.emit()
    return nc


POOL_WINDOWS = (2, 4, 8, 16)
C_U, C_NQ, C_NK, C_NV, C_HQ, C_HF, C_HB, C_HI, C_HG = 0, 1024, 2560, 4096, 5632, 7168, 8704, 10240, 11776


def kmaj(wm):
    K, N = wm.shape
    return np.ascontiguousarray(wm.reshape(K // 128, 128, N).transpose(1, 0, 2))


def pvec(v):
    return np.ascontiguousarray(v.reshape(-1, 128).T)


def rope_perm():
    i = np.arange(128)
    half, li = i // 64, i % 64
    return half * 64 + np.where(li < 32, li + 32, li - 32)


def rope_tables():
    c = np.ones((128, LT), np.float32)
    s = np.zeros((128, LT), np.float32)
    d = np.arange(128)
    half, li = d // 64, d % 64
    inv = (10000.0 ** (-np.arange(0, 64, 2, dtype=np.float32) / 64)).astype(np.float32)
    pos = np.arange(L)
    p = np.where(half[:, None] == 0, (pos // GRID)[None, :], (pos % GRID)[None, :]).astype(np.float32)
    ang = (p * inv[li % 32][:, None]).astype(np.float32)
    c[:, LC:] = np.cos(ang)
    sn = np.sin(ang)
    s[:, LC:] = np.where((li < 32)[:, None], -sn, sn)
    return c, s


def pool_band(w):
    band = np.zeros((128, NT1, 3, 128), np.float32)
    for off, n in ((0, LC), (LC, L)):
        t = np.arange(n)
        lo = np.clip(t - w // 2, 0, n)
        hi = np.clip(t + (w - w // 2), 0, n)
        inv = (1.0 / (hi - lo).astype(np.float32)).astype(np.float32)
        for o in range(-(w // 2), w - w // 2):
            s = t + o
            ok = (s >= 0) & (s < n)
            gt, gs = off + t[ok], off + s[ok]
            np.add.at(band, (gs % 128, gt // 128, gs // 128 - gt // 128 + 1, gt % 128), inv[ok])
        gt = off + t
        np.add.at(band, (gt % 128, gt // 128, 1, gt % 128), -1.0)
    return band


def na_bias_table(rpb_h):
    out = np.full((3, 128, 8, 4, 64), NEG, np.float32)
    kk = np.arange(128)[:, None, None]
    c = np.arange(4)[None, :, None]
    qc = np.arange(64)[None, None, :]
    ko = 128 * c + kk
    krow, kcol = ko // 64, ko % 64
    c0 = np.clip(qc - 8, 0, 48)
    valid = (kcol >= c0) & (kcol < c0 + 16)
    c_off = np.clip(kcol - qc + 15, 0, 30)
    for pat in range(8):
        r = pat if pat <= 4 else 56 + pat
        rs0 = min(max(r - 4, 0), 56)
        r_off = np.broadcast_to(rs0 + krow - r + 7, valid.shape)
        for j in range(3):
            g = rpb_h[j][r_off, np.broadcast_to(c_off, valid.shape)]
            out[j, :, pat, :, :] = np.where(valid, g, NEG)
    return out


_consts = {}


def consts():
    if not _consts:
        _consts["rope"] = rope_tables()
        _consts["band"] = [pool_band(w) for w in POOL_WINDOWS]
        s = np.arange(64)[:, None]
        t = np.arange(64)[None, :]
        _consts["cmask"] = np.ascontiguousarray(np.stack([(s <= t), (s >= t)], axis=1).astype(np.float32))
        _consts["ident"] = np.eye(128, dtype=np.float32)
    return _consts


def mixer_inputs(l, hfull, mod_l, inp):
    cs = consts()
    w_in = inp["w_in"][l]
    perm = rope_perm()
    maps = []
    wq = {}
    for q in range(4):
        cols_tok = [w_in[:, C_U + 256 * q:C_U + 256 * (q + 1)]]
        cols_tok += [w_in[:, C_NV + 384 * q:C_NV + 384 * (q + 1)], w_in[:, C_HI + 384 * q:C_HI + 384 * (q + 1)]]
        feat = []
        for j in range(3):
            hh = 3 * q + j
            wq_ = w_in[:, C_NQ + 128 * hh:C_NQ + 128 * (hh + 1)]
            wk_ = w_in[:, C_NK + 128 * hh:C_NK + 128 * (hh + 1)]
            feat += [wq_, wq_[:, perm], wk_, wk_[:, perm]]
        for j in range(3):
            hh = 3 * q + j
            feat += [w_in[:, c0 + 128 * hh:c0 + 128 * (hh + 1)] for c0 in (C_HQ, C_HF, C_HB, C_HG)]
        hs = slice(384 * q, 384 * (q + 1))
        hgv = np.zeros((128, 3, 8), np.float32)
        hgv[:, :, 0] = pvec(inp["hg_lb"][0, 0, hs])
        hgv[:, :, 1] = pvec(inp["hg_lb"][0, 1, hs])
        hgv[:, :, 2] = pvec(inp["hg_lb"][1, 0, hs])
        hgv[:, :, 3] = pvec(inp["hg_lb"][1, 1, hs])
        hgv[:, :, 4] = pvec(inp["hg_norm_g"][l, hs])
        wq[q] = dict(
            w_tok=kmaj(np.concatenate(cols_tok, axis=1)), w_feat=kmaj(np.concatenate(feat, axis=1)),
            band=cs["band"][q],
            pool_w=np.ascontiguousarray(inp["pool_w"][l, q].reshape(2, 128, 256).transpose(1, 0, 2)),
            pool_s=pvec(inp["pool_scale"][l, 256 * q:256 * (q + 1)]),
            na_bias=na_bias_table(inp["na_rpb"][l, 3 * q:3 * q + 3]), hg_vec=hgv)
    for b in range(B):
        modv = np.stack([pvec(mod_l[b, D:2 * D]), pvec(mod_l[b, 0:D]), pvec(mod_l[2, D:2 * D]), pvec(mod_l[2, 0:D])], axis=1)
        for q in range(4):
            m = dict(wq[q])
            m.update(h=hfull[b], modv=np.ascontiguousarray(modv), rope_c=cs["rope"][0], rope_s=cs["rope"][1],
                     cmask=cs["cmask"], ident=cs["ident"])
            maps.append(m)
    return maps


def mix_gather(res):
    out = []
    for b in range(B):
        full = np.empty((32, 128, LT), NPBF)
        for q in range(4):
            m = res[b * 4 + q]["mixT"]
            full[2 * q:2 * q + 2] = m[0:2]
            full[8 + 3 * q:8 + 3 * q + 3] = m[2:5]
            full[20 + 3 * q:20 + 3 * q + 3] = m[5:8]
        out.append(full)
    return out


_prog_cache = {}


def run(key, builder, in_maps):
    if key not in _prog_cache:
        _prog_cache[key] = builder()
    nc = _prog_cache[key]
    res = run_bass_kernel_spmd(nc, in_maps, core_ids=list(range(NCORES)))
    return res.results


def build_post(NL, NCX, router=True):
    nc = new_nc()
    NTOK = NL + NCX
    mixT_d = din(nc, "mixT", [128, KC, NTOK], BF16)
    wout_d = din(nc, "w_out", [128, KC, D])
    h_d = din(nc, "h", [NTOK, D])
    vbc_d = din(nc, "vbc", [4, D])
    modv_d = din(nc, "modv", [128, 4, KC])
    rw_d = din(nc, "rw", [128, KC, NE])
    rb_d = din(nc, "rb", [1, NE])
    ident_d = din(nc, "ident", [128, 128])
    h1_d = dout(nc, "h1", [NTOK, D])
    fxT_d = dout(nc, "fxT", [128, KC, NTOK], BF16)
    gates_d = dout(nc, "gates", [NTOK, NE])
    y_d = dscr(nc, "ysc", [NTOK, D], F32)
    with ExitStack() as st:
        P = Prog(nc, st)
        with ExitStack() as ph:
            G = GemmBufs(P, ph)
            Sf = Stager(P, ph, "sgf", [128, 512], F32)
            cnt = [0]

            def evac(ps, pk, t0, tsz, n0):
                sg, sk = Sf.next()
                cnt[0] += 1
                evac_copy(P, cnt[0], sg[:tsz, :], ps[:tsz, :], [pk], [sk])
                Sf.store(y_d[t0:t0 + tsz, n0:n0 + 512], sg[:tsz, :], sk, "ysc")

            gemm_stream(P, G, mixT_d, "mixT", NTOK, wout_d, D, "tok", evac)
        P.barrier()
        with ExitStack() as ph:
            ident = P.sb("ident", [128, 128], F32, ph)
            vbc = P.sb("vbc", [128, 4, D], F32, ph)
            modv = P.sb("modv", [128, 4, KC], F32, ph)
            onep = P.sb("onep", [128, 2, KC], F32, ph)
            rw = P.sb("rw", [128, KC, NE], F32, ph)
            rb = P.sb("rb", [128, NE], F32, ph)
            yt = [P.sb(f"yt{i}", [128, D], F32, ph) for i in range(2)]
            hh = [P.sb(f"hh{i}", [128, D], F32, ph) for i in range(2)]
            xs32 = P.sb("xs32", [128, KC, 128], F32, ph)
            xsb = P.sb("xsb", [128, KC, 128], BF16, ph)
            stats = P.sb("stats", [128, 8, 6], F32, ph)
            mv = P.sb("mv", [128, 2], F32, ph)
            rstd = P.sb("rstd", [128, 1], F32, ph)
            lg = P.sb("lg", [128, NE], F32, ph)
            mx8 = P.sb("mx8", [128, 8], F32, ph)
            negm = P.sb("negm", [128, 1], F32, ph)
            msk = P.sb("msk", [128, NE], F32, ph)
            ex = P.sb("ex", [128, NE], F32, ph)
            ssum = P.sb("ssum", [128, 1], F32, ph)
            gt = P.sb("gt", [128, NE], F32, ph)
            pts = [P.ps(f"pt{i}", [128, 4, 128], F32, ph) for i in range(4)]
            prt = P.ps("prt", [128, 512], F32, ph)
            P.dma("sp", ident[:], ident_d[:, :], writes=["ident"])
            for i in range(4):
                P.dma("sp", vbc[:, i, :], vbc_d[i:i + 1, :].partition_broadcast(128), writes=[f"vbc{i}"])
            P.dma("sp", modv[:], modv_d[:, :, :], writes=["modv"])
            P.dma("sp", rw[:], rw_d[:, :, :], writes=["rw"])
            P.dma("sp", rb[:], rb_d[0:1, :].partition_broadcast(128), writes=["rb"])
            P.group("dve", [lambda e: e.tensor_scalar_add(onep[:, 0, :], modv[:, 0, :], 1.0),
                            lambda e: e.tensor_scalar_add(onep[:, 1, :], modv[:, 2, :], 1.0)],
                    reads=["modv"], writes=["onep"])
            tiles = [(t0, 128, 0) for t0 in range(0, NL, 128)]
            if NCX:
                tiles.append((NL, NCX, 1))
            for ti, (t0, rows, w) in enumerate(tiles):
                y, yk = yt[ti % 2], f"yt{ti % 2}"
                x, xk = hh[ti % 2], f"hh{ti % 2}"
                P.dma("sp", y[:rows, :], y_d[t0:t0 + rows, :], reads=["ysc"], writes=[yk], sem="L" + yk)
                P.dma("sp", x[:rows, :], h_d[t0:t0 + rows, :], writes=[xk], sem="L" + xk)
                P.op("pool", lambda e, y=y, rows=rows, w=w: e.tensor_tensor(y[:rows, :], y[:rows, :], vbc[:rows, w, :], ALU.mult),
                     reads=[yk, f"vbc{w}"], writes=[yk])
                P.op("dve", lambda e, y=y, x=x, rows=rows: e.scalar_tensor_tensor(x[:rows, :], x[:rows, :], ALPHA, y[:rows, :], ALU.mult, ALU.add),
                     reads=[yk, xk], writes=[xk])
                ln_stats(P, x, xk, stats, mv, rstd, "lnst", rows)
                P.op("dve", lambda e, x=x, rows=rows: e.tensor_scalar(x[:rows, :], x[:rows, :], mv[:rows, 0:1], rstd[:rows, 0:1], ALU.subtract, ALU.mult),
                     reads=[xk, "lnst"], writes=[xk])
                P.op("pool", lambda e, x=x, rows=rows: e.tensor_tensor(x[:rows, :], x[:rows, :], vbc[:rows, 2, :], ALU.mult),
                     reads=[xk, "vbc2"], writes=[xk])
                P.op("dve", lambda e, x=x, rows=rows: e.tensor_tensor(x[:rows, :], x[:rows, :], vbc[:rows, 3, :], ALU.add),
                     reads=[xk, "vbc3"], writes=[xk])
                P.dma("sp", h1_d[t0:t0 + rows, :], x[:rows, :], reads=[xk], sem="S" + xk)
                ln_stats(P, x, xk, stats, mv, rstd, "lnst", rows)
                P.op("dve", lambda e, x=x, y=y, rows=rows: e.tensor_scalar(y[:rows, :], x[:rows, :], mv[:rows, 0:1], rstd[:rows, 0:1], ALU.subtract, ALU.mult),
                     reads=[xk, "lnst"], writes=[yk])
                keys = [f"xs32_{g}" for g in range(8)]
                transpose_mod(P, y, yk, ident, pts, "pt", onep[:, w, :], modv[:, 2 * w + 1, :], "onep", xs32, keys, rows)
                P.op("pool", lambda e, rows=rows: e.tensor_copy(xsb[:, :, :rows], xs32[:, :, :rows]), reads=keys, writes=["xsb"])
                P.dma("sp", fxT_d[:, :, t0:t0 + rows], xsb[:, :, :rows], reads=["xsb"], sem="Sxsb")
                if not router:
                    continue
                fns = [(lambda e, kc=kc, rows=rows: e.matmul(prt[:rows, 0:NE], xs32[:, kc, :rows], rw[:, kc, :],
                                                            start=(kc == 0), stop=(kc == KC - 1))) for kc in range(KC)]
                P.group("pe", fns, reads=keys + ["rw"], writes=["prt"])
                fns = [lambda e, rows=rows: e.tensor_tensor(lg[:rows, :], prt[:rows, 0:NE], rb[:rows, :], ALU.add),
                       lambda e, rows=rows: e.tensor_copy(ex[:rows, :], lg[:rows, :])]
                for rnd in range(4):
                    fns.append(lambda e, rows=rows: e.tensor_reduce(mx8[:rows, 0:1], ex[:rows, :], AX.X, ALU.max))
                    if rnd == 0:
                        fns.append(lambda e, rows=rows: e.tensor_scalar(negm[:rows, :], mx8[:rows, 0:1], -1.0, None, ALU.mult))
                    if rnd < 3:
                        fns.append(lambda e, rows=rows: e.tensor_scalar(msk[:rows, :], ex[:rows, :], mx8[:rows, 0:1], None, ALU.is_ge))
                        fns.append(lambda e, rows=rows: e.scalar_tensor_tensor(ex[:rows, :], msk[:rows, :], -1e30, ex[:rows, :], ALU.mult, ALU.add))
                fns.append(lambda e, rows=rows: e.tensor_scalar(msk[:rows, :], lg[:rows, :], mx8[:rows, 0:1], None, ALU.is_ge))
                P.group("dve", fns, reads=["prt", "rb"], writes=["lg", "msk", "negm", "ex"])
                P.op("act", lambda e, rows=rows: e.activation(ex[:rows, :], lg[:rows, :], AF.Exp, bias=negm[:rows, 0:1], scale=1.0),
                     reads=["lg", "negm", "ex"], writes=["ex"])
                P.group("dve", [
                    lambda e, rows=rows: e.tensor_tensor(ex[:rows, :], ex[:rows, :], msk[:rows, :], ALU.mult),
                    lambda e, rows=rows: e.reduce_sum(ssum[:rows, :], ex[:rows, :], AX.X),
                    lambda e, rows=rows: e.reciprocal(ssum[:rows, :], ssum[:rows, :]),
                    lambda e, rows=rows: e.tensor_scalar(gt[:rows, :], ex[:rows, :], ssum[:rows, 0:1], None, ALU.mult)],
                    reads=["ex", "msk"], writes=["ex", "gt"])
                P.dma("sp", gates_d[t0:t0 + rows, :], gt[:rows, :], reads=["gt"], sem="Sgt")
        P.emit()
    return nc


def build_moe(TT):
    nc = new_nc()
    fxT_d = din(nc, "fxT", [128, KC, TT], BF16)
    gT_d = din(nc, "gT", [4, TT])
    w1_d = din(nc, "w1", [4, 128, KC, 2 * DE])
    b1_d = din(nc, "b1", [128, 4, 8])
    w2_d = din(nc, "w2", [128, 16, D])
    b2_d = din(nc, "b2", [4, D])
    out_d = dout(nc, "part", [TT, D])
    with ExitStack() as st:
        P = Prog(nc, st)
        xb = [P.sb(f"xb{i}", [128, KC, 512], BF16) for i in range(2)]
        w1b = [P.sb(f"w1b{i}", [128, KC, 2, 128], BF16) for i in range(2)]
        w2b = [P.sb(f"w2b{i}", [128, 16, 512], BF16) for i in range(2)]
        b2b = [P.sb(f"b2b{i}", [4, 512], BF16) for i in range(2)]
        gbc = [P.sb(f"gbc{i}", [128, 4, 512], F32) for i in range(2)]
        g4 = [P.sb(f"g4{i}", [4, 512], F32) for i in range(2)]
        g4b = [P.sb(f"g4b{i}", [4, 512], BF16) for i in range(2)]
        b1 = P.sb("b1", [128, 4, 8], F32)
        hd_ = [P.sb(f"hdn{i}", [128, 16, 512], BF16) for i in range(2)]
        tg = [P.sb(f"tg{i}", [128, 512], F32) for i in range(2)]
        tsg = [P.sb(f"tsg{i}", [128, 512], F32) for i in range(2)]
        tu = [P.sb(f"tu{i}", [128, 512], F32) for i in range(2)]
        S = Stager(P, st, "stg", [128, 512], F32)
        psg = [P.ps(f"psg{i}", [128, 512], F32) for i in range(2)]
        psu = [P.ps(f"psu{i}", [128, 512], F32) for i in range(2)]
        pso = [P.ps(f"pso{i}", [128, 512], F32) for i in range(4)]
        P.dma("sp", b1[:], b1_d[:, :, :], writes=["b1"])
        wi = 0
        w2i = 0
        oi = 0
        for bi, (b0, bsz) in enumerate(token_blocks(TT)):
            i2 = bi % 2
            xt, xk = xb[i2], f"xb{i2}"
            P.dma("pool", xt[:, :, :bsz], fxT_d[:, :, b0:b0 + bsz], writes=[xk], sem="L" + xk)
            for e4 in range(4):
                P.dma("pool", gbc[i2][:, e4, :bsz], gT_d[e4:e4 + 1, b0:b0 + bsz].partition_broadcast(128),
                      writes=[f"gbc{i2}"] if e4 == 0 else [], sem=f"Lgbc{i2}")
            P.last_write[f"gbc{i2}"] = (f"Lgbc{i2}", P.count[f"Lgbc{i2}"])
            P.dma("pool", g4[i2][:, :bsz], gT_d[:, b0:b0 + bsz], writes=[f"g4{i2}"], sem=f"Lg4{i2}")
            P.op("dve", lambda e, i2=i2, bsz=bsz: e.tensor_copy(g4b[i2][:, :bsz], g4[i2][:, :bsz]),
                 reads=[f"g4{i2}"], writes=[f"g4b{i2}"])
            hdn, hk = hd_[i2], f"hdn{i2}"
            for e4 in range(4):
                for j in range(4):
                    wt, wk = w1b[wi % 2], f"w1b{wi % 2}"
                    wi += 1
                    P.dma("pool", wt[:, :, 0, :], w1_d[e4, :, :, j * 128:(j + 1) * 128], writes=[wk], sem="L" + wk)
                    P.dma("pool", wt[:, :, 1, :], w1_d[e4, :, :, DE + j * 128:DE + (j + 1) * 128], writes=[], sem="L" + wk)
                    P.last_write[wk] = ("L" + wk, P.count["L" + wk])
                    k2 = (e4 * 4 + j) % 2
                    pg, pu = psg[k2], psu[k2]
                    fns = [(lambda e, pg=pg, wt=wt, xt=xt, kc=kc, bsz=bsz: e.matmul(
                        pg[:, :bsz], wt[:, kc, 0, :], xt[:, kc, :bsz], start=(kc == 0), stop=(kc == KC - 1))) for kc in range(KC)]
                    fns += [(lambda e, pu=pu, wt=wt, xt=xt, kc=kc, bsz=bsz: e.matmul(
                        pu[:, :bsz], wt[:, kc, 1, :], xt[:, kc, :bsz], start=(kc == 0), stop=(kc == KC - 1))) for kc in range(KC)]
                    P.group("pe", fns, reads=[xk, wk], writes=[f"psg{k2}", f"psu{k2}"])
                    a, s_, u = tg[k2], tsg[k2], tu[k2]
                    bg = b1[:, e4, j:j + 1]
                    bu = b1[:, e4, 4 + j:5 + j]
                    P.op("dve", lambda e, a=a, pg=pg, bg=bg, bsz=bsz: e.tensor_scalar(a[:, :bsz], pg[:, :bsz], bg, 7.0, ALU.add, ALU.min),
                         reads=[f"psg{k2}", "b1"], writes=[f"tg{k2}"])
                    P.op("act", lambda e, a=a, s_=s_, bsz=bsz: e.activation(s_[:, :bsz], a[:, :bsz], AF.Sigmoid, scale=1.702),
                         reads=[f"tg{k2}"], writes=[f"tsg{k2}"])
                    P.group("dve", [
                        lambda e, u=u, pu=pu, bu=bu, bsz=bsz: e.tensor_scalar(u[:, :bsz], pu[:, :bsz], bu, 7.0, ALU.add, ALU.min),
                        lambda e, u=u, bsz=bsz: e.tensor_scalar(u[:, :bsz], u[:, :bsz], -7.0, 1.0, ALU.max, ALU.add)],
                        reads=[f"psu{k2}", "b1"], writes=[f"tu{k2}"])
                    P.group("dve", [
                        lambda e, a=a, s_=s_, bsz=bsz: e.tensor_tensor(a[:, :bsz], a[:, :bsz], s_[:, :bsz], ALU.mult),
                        lambda e, a=a, u=u, bsz=bsz: e.tensor_tensor(a[:, :bsz], a[:, :bsz], u[:, :bsz], ALU.mult),
                        lambda e, a=a, hdn=hdn, e4=e4, j=j, i2=i2, bsz=bsz: e.tensor_tensor(
                            hdn[:, e4 * 4 + j, :bsz], a[:, :bsz], gbc[i2][:, e4, :bsz], ALU.mult)],
                        reads=[f"tg{k2}", f"tsg{k2}", f"tu{k2}", f"gbc{i2}"], writes=[f"tg{k2}", f"{hk}_{e4 * 4 + j}"])
            hkeys = [f"{hk}_{i}" for i in range(16)]
            for nb in range(8):
                wt, wk = w2b[w2i % 2], f"w2b{w2i % 2}"
                bt, bk = b2b[w2i % 2], f"b2b{w2i % 2}"
                w2i += 1
                load_w(P, wt, wk, w2_d[:, :, nb * 512:(nb + 1) * 512], 16)
                P.dma("pool", bt[:, :], b2_d[:, nb * 512:(nb + 1) * 512], writes=[bk], sem="L" + bk)
                for (t0, tsz) in token_tiles(bsz):
                    ps, pk = pso[oi % 4], f"pso{oi % 4}"
                    oi += 1
                    fns = [(lambda e, ps=ps, hdn=hdn, wt=wt, c=c, t0=t0, tsz=tsz: e.matmul(
                        ps[:tsz, :], hdn[:, c, t0:t0 + tsz], wt[:, c, :], start=(c == 0), stop=False)) for c in range(16)]
                    fns.append(lambda e, ps=ps, bt=bt, i2=i2, t0=t0, tsz=tsz: e.matmul(
                        ps[:tsz, :], g4b[i2][:, t0:t0 + tsz], bt[:, :], start=False, stop=True))
                    P.group("pe", fns, reads=hkeys + [wk, bk, f"g4b{i2}"], writes=[pk])
                    sg, sk = S.next()
                    evac_copy(P, oi, sg[:tsz, :], ps[:tsz, :], [pk], [sk])
                    S.store(out_d[b0 + t0:b0 + t0 + tsz, nb * 512:(nb + 1) * 512], sg[:tsz, :], sk)
        P.emit()
    return nc


def build_comb(NL, NCX):
    nc = new_nc()
    NTOK = NL + NCX
    part_d = din(nc, "parts", [NCORES, NTOK, D])
    h_d = din(nc, "h", [NTOK, D])
    vbc_d = din(nc, "vbc", [4, D])
    out_d = dout(nc, "h2", [NTOK, D])
    with ExitStack() as st:
        P = Prog(nc, st)
        vbc = P.sb("vbc", [128, 4, D], F32)
        acc = [P.sb(f"acc{i}", [128, D], F32) for i in range(2)]
        pt = [P.sb(f"pt{i}", [128, D], F32) for i in range(3)]
        stats = P.sb("stats", [128, 8, 6], F32)
        mv = P.sb("mv", [128, 2], F32)
        rstd = P.sb("rstd", [128, 1], F32)
        for i in range(4):
            P.dma("sp", vbc[:, i, :], vbc_d[i:i + 1, :].partition_broadcast(128), writes=[f"vbc{i}"])
        tiles = [(t0, 128, 0) for t0 in range(0, NL, 128)]
        if NCX:
            tiles.append((NL, NCX, 1))
        pi = 0
        for ti, (t0, rows, w) in enumerate(tiles):
            a, ak = acc[ti % 2], f"acc{ti % 2}"
            P.dma("sp", a[:rows, :], part_d[0, t0:t0 + rows, :], writes=[ak], sem="L" + ak)
            for c in range(1, NCORES):
                p_, pk = pt[pi % 3], f"pt{pi % 3}"
                pi += 1
                P.dma("sp", p_[:rows, :], part_d[c, t0:t0 + rows, :], writes=[pk], sem="L" + pk)
                P.op("dve" if c % 2 else "pool", lambda e, a=a, p_=p_, rows=rows: e.tensor_tensor(a[:rows, :], a[:rows, :], p_[:rows, :], ALU.add),
                     reads=[ak, pk], writes=[ak])
            p_, pk = pt[pi % 3], f"pt{pi % 3}"
            pi += 1
            P.dma("sp", p_[:rows, :], h_d[t0:t0 + rows, :], writes=[pk], sem="L" + pk)
            P.op("pool", lambda e, a=a, rows=rows, w=w: e.tensor_tensor(a[:rows, :], a[:rows, :], vbc[:rows, w, :], ALU.mult),
                 reads=[ak, f"vbc{w}"], writes=[ak])
            P.op("dve", lambda e, a=a, p_=p_, rows=rows: e.scalar_tensor_tensor(a[:rows, :], p_[:rows, :], ALPHA, a[:rows, :], ALU.mult, ALU.add),
                 reads=[ak, pk], writes=[ak])
            ln_stats(P, a, ak, stats, mv, rstd, "lnst", rows)
            P.op("dve", lambda e, a=a, rows=rows: e.tensor_scalar(a[:rows, :], a[:rows, :], mv[:rows, 0:1], rstd[:rows, 0:1], ALU.subtract, ALU.mult),
                 reads=[ak, "lnst"], writes=[ak])
            P.op("pool", lambda e, a=a, rows=rows: e.tensor_tensor(a[:rows, :], a[:rows, :], vbc[:rows, 2, :], ALU.mult),
                 reads=[ak, "vbc2"], writes=[ak])
            P.op("dve", lambda e, a=a, rows=rows: e.tensor_tensor(a[:rows, :], a[:rows, :], vbc[:rows, 3, :], ALU.add),
                 reads=[ak, "vbc3"], writes=[ak])
            P.dma("sp", out_d[t0:t0 + rows, :], a[:rows, :], reads=[ak], sem="S" + ak)
        P.emit()
    return nc


def kernel(x, c, ctx, c_ctx, w_mod, b_mod, w_in, pool_w, pool_scale, na_rpb, hg_lb, hg_norm_g,
           w_out, ln1_g, ln1_b, ln2_g, ln2_b, router_w, router_b, exp_w1, exp_b1, exp_w2, exp_b2):
    f = lambda a: np.asarray(a, dtype=np.float32)
    x, c, ctx, c_ctx, w_mod, b_mod, w_in = f(x), f(c), f(ctx), f(c_ctx), f(w_mod), f(b_mod), f(w_in)
    w_out, ln1_g, ln1_b, ln2_g, ln2_b = f(w_out), f(ln1_g), f(ln1_b), f(ln2_g), f(ln2_b)
    router_w, router_b, exp_w1, exp_b1, exp_w2, exp_b2 = f(router_w), f(router_b), f(exp_w1), f(exp_b1), f(exp_w2), f(exp_b2)
    inp = dict(w_in=w_in, pool_w=f(pool_w), pool_scale=f(pool_scale), na_rpb=f(na_rpb), hg_lb=f(hg_lb), hg_norm_g=f(hg_norm_g))
    cs = consts()
    cond = np.concatenate([c, c_ctx[None]], 0)
    condT = kmaj(np.ascontiguousarray(cond.T))
    NP = 6 * D // 4
    maps = []
    for i in range(NCORES):
        l, k = i // 4, i % 4
        maps.append(dict(xT=condT, w=kmaj(w_mod[l][:, k * NP:(k + 1) * NP]),
                         bias=np.ascontiguousarray(b_mod[l][None, k * NP:(k + 1) * NP])))
    res = run(("mod",), lambda: build_mod(NP), maps)
    mods = [np.concatenate([res[4 * l + k]["out"] for k in range(4)], axis=1) for l in range(DEPTH)]
    h = [np.array(x[b]) for b in range(B)]
    hc = [np.array(ctx[b]) for b in range(B)]
    for l in range(DEPTH):
        ctx_out = l < DEPTH - 1
        mod_l = mods[l]
        hfull = [np.concatenate([hc[b], h[b]], 0) for b in range(B)]
        res = run(("mix", l), lambda: build_mixer(l, ctx_out), mixer_inputs(l, hfull, mod_l, inp))
        mixfull = mix_gather(res)
        del res, hfull
        NL, NCX = L // 4, (LC // 4 if ctx_out else 0)
        NTOK = NL + NCX
        wo = kmaj(w_out[l])
        rwk = kmaj(router_w[l])
        maps = []
        for i in range(NCORES):
            b, qq = i // 4, i % 4
            pm = [mixfull[b][:, :, LC + NL * qq:LC + NL * (qq + 1)]]
            phh = [h[b][NL * qq:NL * (qq + 1)]]
            if ctx_out:
                pm.append(mixfull[b][:, :, NCX * qq:NCX * (qq + 1)])
                phh.append(hc[b][NCX * qq:NCX * (qq + 1)])
            mixT = np.ascontiguousarray(np.concatenate(pm, axis=2).transpose(1, 0, 2))
            vbc = np.stack([mod_l[b, 2 * D:3 * D], mod_l[2, 2 * D:3 * D], ln1_g[l], ln1_b[l]])
            modv = np.stack([pvec(mod_l[b, 4 * D:5 * D]), pvec(mod_l[b, 3 * D:4 * D]),
                             pvec(mod_l[2, 4 * D:5 * D]), pvec(mod_l[2, 3 * D:4 * D])], axis=1)
            maps.append(dict(mixT=mixT, w_out=wo, h=np.ascontiguousarray(np.concatenate(phh, 0)), vbc=np.ascontiguousarray(vbc),
                             modv=np.ascontiguousarray(modv), rw=rwk, rb=np.ascontiguousarray(router_b[l][None]), ident=cs["ident"]))
        res2 = run(("post", NL, NCX), lambda: build_post(NL, NCX), maps)
        del mixfull
        TT = NCORES * NTOK
        fxT = np.ascontiguousarray(np.concatenate([r["fxT"] for r in res2], axis=2))
        gates = np.concatenate([r["gates"] for r in res2], axis=0)
        maps = []
        for e in range(NCORES):
            es = slice(4 * e, 4 * e + 4)
            w1k = np.ascontiguousarray(exp_w1[l, es].reshape(4, KC, 128, 2 * DE).transpose(0, 2, 1, 3))
            b1k = np.ascontiguousarray(exp_b1[l, es].reshape(4, 8, 128).transpose(2, 0, 1))
            w2k = np.ascontiguousarray(exp_w2[l, es].reshape(4, 4, 128, D).transpose(2, 0, 1, 3)).reshape(128, 16, D)
            maps.append(dict(fxT=fxT, gT=np.ascontiguousarray(gates[:, es].T), w1=w1k, b1=b1k, w2=w2k,
                             b2=np.ascontiguousarray(exp_b2[l, es])))
        res3 = run(("moe", TT), lambda: build_moe(TT), maps)
        del maps, fxT
        maps = []
        for i in range(NCORES):
            b = i // 4
            parts = np.stack([r["part"][i * NTOK:(i + 1) * NTOK] for r in res3], 0)
            vbc = np.stack([mod_l[b, 5 * D:6 * D], mod_l[2, 5 * D:6 * D], ln2_g[l], ln2_b[l]])
            maps.append(dict(parts=parts, h=res2[i]["h1"], vbc=np.ascontiguousarray(vbc)))
        del res3
        res4 = run(("comb", NL, NCX), lambda: build_comb(NL, NCX), maps)
        del maps
        for i in range(NCORES):
            b, qq = i // 4, i % 4
            h2 = res4[i]["h2"]
            h[b][NL * qq:NL * (qq + 1)] = h2[:NL]
            if ctx_out:
                hc[b][NCX * qq:NCX * (qq + 1)] = h2[NL:]
    return np.stack(h).astype(np.float32)
```
